# Optimizing a Trainium2 kernel written in Bass

```python
import math
import jax, jax.numpy as jnp
from jax import lax
import numpy as np

D_MODEL = 1024
BATCH = 16
SEQ = 2048
DEPTH = 2

CTX_LEN = 256
GRID_W = 64
CHUNK = 128
ROWS_PER_CHUNK = CHUNK // GRID_W
EPS = 1e-6

SSD_HEAD_DIM = 64
SSD_HEADS = D_MODEL // SSD_HEAD_DIM
SSD_WIDTH = SSD_HEADS * SSD_HEAD_DIM
SSD_GROUPS = 4
SSD_STATE = 128
SSD_CONV = 3
DT_MIN = 0.001
DT_MAX = 0.1
GN = SSD_GROUPS * SSD_STATE
N_XBC = SSD_WIDTH + 2 * GN
N_SCAN = N_XBC + 2 * SSD_HEADS

MLP_WIDTH = D_MODEL
MLP_GROUP_DIM = 128
MLP_GROUPS = MLP_WIDTH // MLP_GROUP_DIM

N_IN = N_SCAN + SSD_WIDTH + 2 * MLP_WIDTH + 2 * D_MODEL

N_EXPERTS = 16
EXPERT_FF = D_MODEL
CAPACITY_FACTOR = 2

kernel_name = 'hybrid_ssd_gmlp_ecmoe_dit_prefix'


def split_cols(p, sizes):
    idx = np.cumsum(sizes)[:-1].tolist()
    return jnp.split(p, idx, axis=-1)


def rmsnorm(x, w):
    xf = x.astype(jnp.float32)
    y = xf * lax.rsqrt(jnp.mean(xf * xf, axis=-1, keepdims=True) + EPS)
    return (y * w.astype(jnp.float32)).astype(x.dtype)


def layernorm(x, w, b):
    xf = x.astype(jnp.float32)
    mu = jnp.mean(xf, axis=-1, keepdims=True)
    var = jnp.mean(jnp.square(xf - mu), axis=-1, keepdims=True)
    y = (xf - mu) * lax.rsqrt(var + EPS)
    return (y * w.astype(jnp.float32) + b.astype(jnp.float32)).astype(x.dtype)


def dwconv_centred(x, w, b):
    ch = x.shape[-1]
    k = w.shape[0]
    y = lax.conv_general_dilated(x, w[:, None, :].astype(x.dtype), window_strides=(1,),
                                 padding=[(k // 2, k // 2)],
                                 dimension_numbers=('NWC', 'WIO', 'NWC'),
                                 feature_group_count=ch)
    return y + b.astype(x.dtype)


def ssd_chunked(xh, dt, A, bm, cm, h0):
    b, l, H, P = xh.shape
    G, N = bm.shape[2], bm.shape[3]
    R = H // G
    nc = l // CHUNK
    x = xh.reshape(b, nc, CHUNK, G, R, P)
    dt_c = dt.reshape(b, nc, CHUNK, G, R)
    a_cum = jnp.cumsum(dt_c * A.reshape(G, R), axis=2)
    Bc = bm.reshape(b, nc, CHUNK, G, N)
    Cc = cm.reshape(b, nc, CHUNK, G, N)
    xdt = x * dt_c[..., None].astype(x.dtype)
    lower = jnp.tril(jnp.ones((CHUNK, CHUNK), dtype=bool))[None, None, :, :, None, None]
    seg = a_cum[:, :, :, None] - a_cum[:, :, None, :]
    decay = jnp.exp(jnp.where(lower, seg, -jnp.inf))
    cb = jnp.einsum('bcign,bcjgn->bcijg', Cc, Bc)
    y_diag = jnp.einsum('bcijg,bcijgr,bcjgrp->bcigrp', cb, decay, xdt)
    decay_to_end = jnp.exp(a_cum[:, :, -1:] - a_cum)
    states = jnp.einsum('bcjgn,bcjgr,bcjgrp->bcgrpn', Bc, decay_to_end, xdt)
    chunk_decay = jnp.exp(a_cum[:, :, -1])

    def step(h, inp):
        st, dcy = inp
        return h * dcy[..., None, None] + st, h

    h_final, h_in = lax.scan(step, h0, (jnp.moveaxis(states, 1, 0).astype(jnp.float32),
                                        jnp.moveaxis(chunk_decay, 1, 0)))
    y_off = jnp.einsum('bcign,bcigr,cbgrpn->bcigrp', Cc, jnp.exp(a_cum), h_in)
    y = (y_diag + y_off).reshape(b, l, H, P).astype(xh.dtype)
    return y, h_final


def ssd_scan_bidir(p_scan, conv_w, conv_b, dt_bias, a_log, h0_f, h0_b):
    xbc, dt_raw = p_scan[..., :N_XBC], p_scan[..., N_XBC:]
    xbc = jax.nn.silu(dwconv_centred(xbc, conv_w, conv_b))
    xs, bm, cm = split_cols(xbc, [SSD_WIDTH, GN, GN])
    b, l, _ = xs.shape
    xh = xs.reshape(b, l, SSD_HEADS, SSD_HEAD_DIM)
    bm = bm.reshape(b, l, SSD_GROUPS, SSD_STATE)
    cm = cm.reshape(b, l, SSD_GROUPS, SSD_STATE)
    dt = jax.nn.softplus(dt_raw.astype(jnp.float32).reshape(b, l, 2, SSD_HEADS)
                         + dt_bias.astype(jnp.float32))
    A = -jnp.exp(a_log.astype(jnp.float32))
    y_f, st_f = ssd_chunked(xh, dt[:, :, 0], A[0], bm, cm, h0_f)
    rev = lambda t: jnp.flip(t, axis=1)
    y_b, st_b = ssd_chunked(rev(xh), rev(dt[:, :, 1]), A[1], rev(bm), rev(cm), h0_b)
    return y_f + rev(y_b), xh, st_f, st_b


def ssd_output(y, xh, z, d_skip, norm_w):
    b, l, H, P = xh.shape
    y = y + d_skip.astype(xh.dtype)[:, None] * xh
    g = y.reshape(b, l, SSD_WIDTH) * jax.nn.silu(z)
    g = rmsnorm(g.reshape(b, l, SSD_GROUPS, SSD_WIDTH // SSD_GROUPS),
                norm_w.reshape(SSD_GROUPS, SSD_WIDTH // SSD_GROUPS))
    return g.reshape(b, l, SSD_WIDTH)


def spatial_gating(u, v, n_chunks, ln_w, ln_b, w_s, b_s):
    b, l, _ = v.shape
    v = layernorm(v, ln_w, ln_b)
    vc = v.reshape(b, n_chunks, CHUNK, MLP_GROUPS, MLP_GROUP_DIM)
    mixed = jnp.einsum('gij,bcjgd->bcigd', w_s, vc) + b_s.T[None, None, :, :, None]
    return u * mixed.reshape(b, l, MLP_WIDTH)


def mixer_out(y, xh, p_rest, n_chunks, d_skip, ssd_norm_w, ln_w, ln_b, w_s, b_s, w_pa, w_pb, w_o):
    z, uv, gate_a, gate_b = split_cols(p_rest, [SSD_WIDTH, 2 * MLP_WIDTH, D_MODEL, D_MODEL])
    ssd = ssd_output(y, xh, z, d_skip, ssd_norm_w)
    u, v = jnp.split(jax.nn.gelu(uv), 2, axis=-1)
    sgu = spatial_gating(u, v, n_chunks, ln_w, ln_b, w_s, b_s)
    merged = jax.nn.sigmoid(gate_a) * (ssd @ w_pa) + jax.nn.sigmoid(gate_b) * (sgu @ w_pb)
    return merged @ w_o


def expert_choice_ffn(xn, router_w, w_gate, w_up, w_down):
    b, n, d = xn.shape
    cap = CAPACITY_FACTOR * n // N_EXPERTS
    logits = jnp.einsum('bnd,de->ben', xn, router_w).astype(jnp.float32)
    aff = jax.nn.softmax(logits, axis=1)
    gate, idx = lax.top_k(aff, cap)
    xg = jax.vmap(lambda xs, ix: xs[ix])(xn, idx)
    hid = jax.nn.silu(jnp.einsum('becd,edf->becf', xg, w_gate)) * jnp.einsum('becd,edf->becf', xg, w_up)
    y = jnp.einsum('becf,efd->becd', hid, w_down) * gate[..., None].astype(xn.dtype)
    scatter = lambda ix, ys: jnp.zeros((n, d), ys.dtype).at[ix.reshape(-1)].add(ys.reshape(-1, d))
    return jax.vmap(scatter)(idx, y)


def setup_inputs(seed: int = 0) -> dict:
    key = jax.random.key(seed)
    ks = jax.random.split(key, 32)
    f32 = jnp.float32
    L = DEPTH
    nrm = lambda k, shape, s: jax.random.normal(k, shape, f32) * s
    gain = lambda k, shape: 1.0 + 0.01 * jax.random.normal(k, shape, f32)
    dt0 = jnp.exp(jax.random.uniform(ks[12], (L, 2, SSD_HEADS), f32, math.log(DT_MIN), math.log(DT_MAX)))
    return {
        'x': nrm(ks[0], (BATCH, SEQ, D_MODEL), 1.0),
        'c': nrm(ks[1], (BATCH, D_MODEL), 1.0),
        'ctx': nrm(ks[2], (BATCH, CTX_LEN, D_MODEL), 1.0),
        'c_ctx': nrm(ks[3], (D_MODEL,), 1.0),
        'ada_w': nrm(ks[4], (L, D_MODEL, 6 * D_MODEL), 0.5 * D_MODEL ** -0.5),
        'ada_b': nrm(ks[5], (L, 6 * D_MODEL), 0.01),
        'norm1_w': gain(ks[6], (L, D_MODEL)),
        'norm2_w': gain(ks[7], (L, D_MODEL)),
        'w_in': nrm(ks[8], (L, D_MODEL, N_IN), D_MODEL ** -0.5),
        'conv_w': nrm(ks[9], (L, SSD_CONV, N_XBC), SSD_CONV ** -0.5),
        'conv_b': nrm(ks[10], (L, N_XBC), 0.01),
        'dt_bias': dt0 + jnp.log(-jnp.expm1(-dt0)),
        'a_log': jnp.log(jax.random.uniform(ks[13], (L, 2, SSD_HEADS), f32, 1.0, 16.0)),
        'd_skip': gain(ks[14], (L, SSD_HEADS)),
        'ssd_norm_w': gain(ks[15], (L, SSD_WIDTH)),
        'sgu_ln_w': gain(ks[16], (L, MLP_WIDTH)),
        'sgu_ln_b': nrm(ks[17], (L, MLP_WIDTH), 0.01),
        'w_s': nrm(ks[18], (L, MLP_GROUPS, CHUNK, CHUNK), CHUNK ** -0.5),
        'b_s': gain(ks[19], (L, MLP_GROUPS, CHUNK)),
        'w_pa': nrm(ks[20], (L, SSD_WIDTH, D_MODEL), SSD_WIDTH ** -0.5),
        'w_pb': nrm(ks[21], (L, MLP_WIDTH, D_MODEL), MLP_WIDTH ** -0.5),
        'w_o': nrm(ks[22], (L, D_MODEL, D_MODEL), D_MODEL ** -0.5),
        'router_w': nrm(ks[23], (L, D_MODEL, N_EXPERTS), D_MODEL ** -0.5),
        'w_gate': nrm(ks[24], (L, N_EXPERTS, D_MODEL, EXPERT_FF), D_MODEL ** -0.5),
        'w_up': nrm(ks[25], (L, N_EXPERTS, D_MODEL, EXPERT_FF), D_MODEL ** -0.5),
        'w_down': nrm(ks[26], (L, N_EXPERTS, EXPERT_FF, D_MODEL), EXPERT_FF ** -0.5),
        'final_norm_w': gain(ks[27], (D_MODEL,)),
    }


def reference(x, c, ctx, c_ctx, ada_w, ada_b, norm1_w, norm2_w, w_in, conv_w, conv_b, dt_bias, a_log,
              d_skip, ssd_norm_w, sgu_ln_w, sgu_ln_b, w_s, b_s, w_pa, w_pb, w_o, router_w,
              w_gate, w_up, w_down, final_norm_w):
    bsz, n_lat, _ = x.shape
    rows = n_lat // GRID_W
    lat_chunks = rows // ROWS_PER_CHUNK
    ctx_chunks = ctx.shape[1] // CHUNK
    h0 = jnp.zeros((bsz, SSD_GROUPS, SSD_HEADS // SSD_GROUPS, SSD_HEAD_DIM, SSD_STATE), jnp.float32)
    h, hc = x, ctx
    for layer in range(DEPTH):
        last = layer == DEPTH - 1
        mod_l = (jax.nn.silu(c) @ ada_w[layer] + ada_b[layer])[:, None, :]
        mod_c = (jax.nn.silu(c_ctx) @ ada_w[layer] + ada_b[layer])[None, None, :]
        sh1_l, sc1_l, g1_l, sh2_l, sc2_l, g2_l = jnp.split(mod_l, 6, axis=-1)
        sh1_c, sc1_c, g1_c, sh2_c, sc2_c, g2_c = jnp.split(mod_c, 6, axis=-1)
        w_in_l = w_in[layer]
        mix_args = (d_skip[layer], ssd_norm_w[layer], sgu_ln_w[layer], sgu_ln_b[layer],
                    w_s[layer], b_s[layer], w_pa[layer], w_pb[layer], w_o[layer])
        xm_c = rmsnorm(hc, norm1_w[layer]) * (1 + sc1_c) + sh1_c
        xm_l = rmsnorm(h, norm1_w[layer]) * (1 + sc1_l) + sh1_l
        y_c, xh_c, st_f, st_b = ssd_scan_bidir(xm_c @ w_in_l[:, :N_SCAN], conv_w[layer], conv_b[layer],
                                               dt_bias[layer], a_log[layer], h0, h0)
        p_l = xm_l @ w_in_l
        y_l, xh_l, _, _ = ssd_scan_bidir(p_l[..., :N_SCAN], conv_w[layer], conv_b[layer],
                                         dt_bias[layer], a_log[layer], st_f, st_b)
        h = h + g1_l * mixer_out(y_l, xh_l, p_l[..., N_SCAN:], lat_chunks, *mix_args)
        if not last:
            hc = hc + g1_c * mixer_out(y_c, xh_c, xm_c @ w_in_l[:, N_SCAN:], ctx_chunks, *mix_args)
        moe_args = (router_w[layer], w_gate[layer], w_up[layer], w_down[layer])
        xf_l = rmsnorm(h, norm2_w[layer]) * (1 + sc2_l) + sh2_l
        h = h + g2_l * expert_choice_ffn(xf_l, *moe_args)
        if not last:
            xf_c = rmsnorm(hc, norm2_w[layer]) * (1 + sc2_c) + sh2_c
            hc = hc + g2_c * expert_choice_ffn(xf_c, *moe_args)
    return rmsnorm(h, final_norm_w)
```

```python
import contextlib
import numpy as np
import concourse.bass as bass
import concourse.mybir as mybir
from concourse.bass_utils import run_bass_kernel_spmd

F32 = mybir.dt.float32
BF16 = mybir.dt.bfloat16
AF = mybir.ActivationFunctionType
ALU = mybir.AluOpType
AX = mybir.AxisListType.X

NCORES = 8
D = 1024
KT = 8
T = 128
NLAT = 2048
NCTX = 256
NTOK = NLAT + NCTX
DEPTH = 2
NIN = 7200
NE = 16
EPS = 1e-6
NBIS = 26
POOL_ENG = "dve"
import os
PHASEMAP = os.environ.get("MK_PHASEMAP", "")


class Buf:
    def __init__(self, t, name):
        self.t = t
        self.name = name
        self.last_writer = None
        self.readers = []
        self.sem = None
        self.dma_count = 0
        self.nowaw = False

    def __getitem__(self, idx):
        return V(self, self.t[idx])


class V:
    def __init__(self, buf, ap):
        self.buf = buf
        self.ap = ap

    def __getitem__(self, idx):
        return V(self.buf, self.ap[idx])

    def rr(self, s, **kw):
        return V(self.buf, self.ap.rearrange(s, **kw))

    def bc(self, shape):
        return V(self.buf, self.ap.broadcast_to(list(shape)))

    def us(self, axis):
        return V(self.buf, self.ap.unsqueeze(axis))


def _ap(x):
    return x.ap if isinstance(x, V) else x


def _bufs(*xs):
    out = []
    for x in xs:
        if isinstance(x, V) and x.buf not in out:
            out.append(x.buf)
    return out


class Op:
    __slots__ = ("eng", "fn", "is_dma", "waits", "signal", "count", "dst", "idx", "slot", "epoch", "phase")


class Sched:
    ENG = ("pe", "act", "dve", "pool", "sp")

    def __init__(self, nc):
        self.nc = nc
        self.ops = []
        self.per_eng = {e: [] for e in self.ENG}
        self.pending_barrier = {e: None for e in self.ENG}
        self.epoch = 0
        self.slot_count = []
        self.epoch_slots = {}
        self.phase = "init"
        self.phase_map = {}

    def _slot(self, buf):
        if buf not in self.epoch_slots:
            i = len(self.epoch_slots)
            if i >= len(self.slot_count):
                self.slot_count.append(0)
            self.epoch_slots[buf] = i
        return self.epoch_slots[buf]

    def barrier(self):
        last = {e: (self.per_eng[e][-1] if self.per_eng[e] else None) for e in self.ENG}
        dmas = list(self.slot_count)
        for e in self.ENG:
            self.pending_barrier[e] = (last, dmas)
        self.epoch += 1
        self.epoch_slots = {}

    def op(self, eng, fn, reads=(), writes=(), dma=False):
        o = Op()
        o.eng = eng; o.fn = fn; o.is_dma = dma; o.waits = []; o.signal = False
        o.count = None; o.idx = len(self.ops); o.dst = None; o.slot = None; o.epoch = self.epoch
        o.phase = self.phase
        deps = []
        for b in reads:
            if b.last_writer is not None:
                deps.append((b.last_writer, "raw"))
        for b in writes:
            if b.last_writer is not None and not (dma and b.nowaw and b.last_writer.is_dma):
                deps.append((b.last_writer, "waw"))
            for r in b.readers:
                deps.append((r, "war"))
        if dma:
            assert len(writes) == 1
            o.dst = writes[0]
            o.slot = self._slot(o.dst)
            self.slot_count[o.slot] += 1
        pb = self.pending_barrier[eng]
        if pb is not None:
            last, dmas = pb
            for e2, lo in last.items():
                if lo is not None and not lo.is_dma:
                    if e2 == eng and not dma:
                        continue
                    lo.signal = True
                    o.waits.append(("eng", lo))
            for i, cnt in enumerate(dmas):
                if cnt > 0:
                    o.waits.append(("dma", i, cnt))
            self.pending_barrier[eng] = None
        for (d, kind) in deps:
            if d is o:
                continue
            if d.is_dma:
                if d.epoch != self.epoch:
                    continue
                cnt = self.slot_count[d.slot] - (1 if (dma and d.slot == o.slot) else 0)
                o.waits.append(("dma", d.slot, cnt))
            else:
                if d.eng == eng and not dma:
                    if eng == "pe":
                        continue
                    if kind != "raw":
                        continue
                d.signal = True
                o.waits.append(("eng", d))
        for b in writes:
            b.last_writer = o
            b.readers = []
        for b in reads:
            if b not in writes:
                b.readers.append(o)
        self.ops.append(o)
        self.per_eng[eng].append(o)
        return o

    def mm(self, out, lhsT, rhs, start=True, stop=True):
        self.op("pe", lambda e: e.matmul(out=_ap(out), lhsT=_ap(lhsT), rhs=_ap(rhs), start=start, stop=stop),
                reads=_bufs(lhsT, rhs), writes=_bufs(out))

    def tr(self, out, in_, ident):
        self.op("pe", lambda e: e.transpose(out=_ap(out), in_=_ap(in_), identity=_ap(ident)),
                reads=_bufs(in_, ident), writes=_bufs(out))

    def act(self, out, in_, func, bias=0.0, scale=1.0, accum=None):
        kw = {}
        if accum is not None:
            kw["accum_out"] = _ap(accum)
        self.op("act", lambda e: e.activation(out=_ap(out), in_=_ap(in_), func=func, bias=_ap(bias), scale=_ap(scale), **kw),
                reads=_bufs(in_, bias, scale), writes=_bufs(out, accum))

    def tt(self, out, in0, in1, op, eng="dve"):
        self.op(eng, lambda e: e.tensor_tensor(out=_ap(out), in0=_ap(in0), in1=_ap(in1), op=op),
                reads=_bufs(in0, in1), writes=_bufs(out))

    def ts(self, out, in0, s1, s2=None, op0=ALU.mult, op1=None, accum=None, eng="dve"):
        kw = {}
        if op1 is not None:
            kw["op1"] = op1
        if accum is not None:
            kw["accum_out"] = _ap(accum)
        self.op(eng, lambda e: e.tensor_scalar(out=_ap(out), in0=_ap(in0), scalar1=_ap(s1), scalar2=_ap(s2), op0=op0, **kw),
                reads=_bufs(in0, s1, s2), writes=_bufs(out, accum))

    def stt(self, out, in0, scalar, in1, op0, op1):
        self.op("dve", lambda e: e.scalar_tensor_tensor(out=_ap(out), in0=_ap(in0), scalar=_ap(scalar), in1=_ap(in1), op0=op0, op1=op1),
                reads=_bufs(in0, scalar, in1), writes=_bufs(out))

    def cp(self, out, in_, eng="dve"):
        if eng == "act":
            self.op("act", lambda e: e.activation(out=_ap(out), in_=_ap(in_), func=AF.Copy), reads=_bufs(in_), writes=_bufs(out))
        else:
            self.op(eng, lambda e: e.tensor_copy(out=_ap(out), in_=_ap(in_)), reads=_bufs(in_), writes=_bufs(out))

    def red(self, out, in_, op):
        self.op("dve", lambda e: e.tensor_reduce(out=_ap(out), in_=_ap(in_), axis=AX, op=op), reads=_bufs(in_), writes=_bufs(out))

    def recip(self, out, in_):
        self.op("dve", lambda e: e.reciprocal(out=_ap(out), in_=_ap(in_)), reads=_bufs(in_), writes=_bufs(out))

    def memset(self, out, val, eng="dve"):
        self.op(eng, lambda e: e.memset(_ap(out), val), writes=_bufs(out))

    def dma(self, out, in_, eng="sp", slow=False):
        kw = {"allow_slow_non_contiguous": True} if slow else {}
        self.op(eng, lambda e: e.dma_start(out=_ap(out), in_=_ap(in_), **kw), reads=_bufs(in_), writes=_bufs(out), dma=True)

    def emit(self, final_waits=()):
        nc = self.nc
        with contextlib.ExitStack() as st:
            esem = {e: st.enter_context(nc.semaphore("s_" + e)) for e in self.ENG}
            dsem = [st.enter_context(nc.semaphore("d_%d" % i)) for i in range(len(self.slot_count))]
            for e in self.ENG:
                c = 0
                for o in self.per_eng[e]:
                    if o.signal and not o.is_dma:
                        c += 1
                        o.count = c
            block = st.enter_context(nc.Block())
            engh = {"pe": block.tensor, "act": block.scalar, "dve": block.vector,
                    "pool": block.gpsimd, "sp": block.sync}

            def mk(e):
                def body(eng):
                    known = {}
                    for o in self.per_eng[e]:
                        need = {}
                        for w in o.waits:
                            if w[0] == "dma":
                                s = dsem[w[1]]; v = 16 * w[2]
                            else:
                                s = esem[w[1].eng]; v = w[1].count
                            if v <= 0:
                                continue
                            k = id(s)
                            if k not in need or need[k][1] < v:
                                need[k] = (s, v)
                        for k, (s, v) in need.items():
                            if known.get(k, 0) >= v:
                                continue
                            eng.wait_ge(s, v)
                            known[k] = v
                        ins = o.fn(eng)
                        if PHASEMAP:
                            try:
                                self.phase_map[ins.ins.name] = o.phase
                            except Exception:
                                pass
                        if o.is_dma:
                            ins.then_inc(dsem[o.slot], 16)
                        elif o.signal:
                            ins.then_inc(esem[e], 1)
                    if e == "sp":
                        for i, cnt in enumerate(self.slot_count):
                            if cnt > 0:
                                eng.wait_ge(dsem[i], 16 * cnt)
                return body

            for e in self.ENG:
                engh[e](mk(e))
        if PHASEMAP:
            import json
            json.dump(self.phase_map, open(PHASEMAP, "w"))
        print("sched: ops", len(self.ops), "dma slots", len(self.slot_count), {e: len(v) for e, v in self.per_eng.items()}, flush=True)


class Ring:
    def __init__(self, bufs):
        self.bufs = bufs
        self.i = 0

    def next(self):
        b = self.bufs[self.i % len(self.bufs)]
        self.i += 1
        return b


def build_nc(nseq=2, stage="full", nlayers=DEPTH):
    nc = bass.Bass("TRN2", target_bir_lowering=False)
    S = Sched(nc)
    _uid = [0]

    def dram_in(name, shape):
        return Buf(nc.dram_tensor(name, list(shape), F32, kind="ExternalInput").ap(), name)

    def dram_scr(name, shape, dt=F32):
        b = Buf(nc.dram_tensor(name, list(shape), dt, kind="Internal").ap(), name)
        b.nowaw = True
        return b

    x_in = dram_in("x", [nseq, NLAT, D])
    ctx_in = dram_in("ctx", [nseq, NCTX, D])
    cT_in = dram_in("cT", [T, KT, 3])
    ada_w = dram_in("ada_w", [DEPTH, D, 6 * D])
    ada_b = dram_in("ada_b", [DEPTH, 6 * D])
    norm1_w = dram_in("norm1_w", [DEPTH, D])
    norm2_w = dram_in("norm2_w", [DEPTH, D])
    w_in = dram_in("w_in", [DEPTH, D, NIN])
    convw_h = dram_in("convw_h", [DEPTH, T, 16, 3])
    convb_h = dram_in("convb_h", [DEPTH, T, 16])
    dt_bias = dram_in("dt_bias", [DEPTH, 32])
    a_log = dram_in("a_log", [DEPTH, 32])
    d_skip = dram_in("d_skip", [DEPTH, 16])
    ssd_norm_w = dram_in("ssd_norm_w", [DEPTH, D])
    sgu_ln_w = dram_in("sgu_ln_w", [DEPTH, D])
    sgu_ln_b = dram_in("sgu_ln_b", [DEPTH, D])
    w_s = dram_in("w_s", [DEPTH, 8, T, T])
    b_s = dram_in("b_s", [DEPTH, 8 * T])
    w_pa = dram_in("w_pa", [DEPTH, D, D])
    w_pb = dram_in("w_pb", [DEPTH, D, D])
    w_o = dram_in("w_o", [DEPTH, D, D])
    router_w = dram_in("router_w", [DEPTH, D, NE])
    w_gate = dram_in("w_gate", [DEPTH, NE, D, D])
    w_up = dram_in("w_up", [DEPTH, NE, D, D])
    w_down = dram_in("w_down", [DEPTH, NE, D, D])
    final_norm_w = dram_in("final_norm_w", [1, D])
    y_out = Buf(nc.dram_tensor("y", [nseq, NLAT, D], F32, kind="ExternalOutput").ap(), "y")
    y_out.nowaw = True

    mod_d = dram_scr("mod_d", [DEPTH, 3, 6 * D])
    hA = [dram_scr(f"hA{s}", [NTOK, D]) for s in range(nseq)]
    hB = [dram_scr(f"hB{s}", [NTOK, D]) for s in range(nseq)]
    ybF = [dram_scr(f"ybF{s}", [NTOK, D]) for s in range(nseq)]
    ybT = [dram_scr(f"ybT{s}", [NTOK, D]) for s in range(nseq)]
    atb = [dram_scr(f"atb{s}", [NTOK // T, T, KT * T]) for s in range(nseq)]
    ywb = [dram_scr(f"ywb{s}", [NE, 2, T, D], BF16) for s in range(nseq)]
    ywc = [dram_scr(f"ywc{s}", [NE, 1, 32, D], BF16) for s in range(nseq)]
    dbg = {}

    def sb(st, name, shape, dt=F32):
        _uid[0] += 1
        nm = f"{name}_{_uid[0]}"
        return Buf(st.enter_context(nc.sbuf_tensor(nm, list(shape), dt)), nm)

    def ps(st, name, shape, dt=F32):
        _uid[0] += 1
        nm = f"{name}_{_uid[0]}"
        return Buf(st.enter_context(nc.psum_tensor(nm, list(shape), dt)), nm)

    def ring(st, name, shape, dt, n, psum=False):
        return Ring([(ps if psum else sb)(st, f"{name}{i}", shape, dt) for i in range(n)])

    with contextlib.ExitStack() as top:
        identF = sb(top, "identF", [T, T]); identB = sb(top, "identB", [T, T], BF16)
        Um = sb(top, "Um", [T, T]); Vm = sb(top, "Vm", [T, T]); Lf = sb(top, "Lf", [T, T]); Lb = sb(top, "Lb", [T, T])
        onesF = sb(top, "onesF", [T, T]); mhalf = sb(top, "mhalf", [T, 8])
        iota_slot = sb(top, "iota_slot", [T, 256]); iota_part = sb(top, "iota_part", [T, 2])

        def aff_sel(buf_v, pattern, cmp, fill, cm, base=0):
            S.op("pool", lambda e: e.affine_select(out=_ap(buf_v), in_=_ap(buf_v), pattern=pattern, compare_op=cmp,
                                                   fill=fill, base=base, channel_multiplier=cm),
                 reads=_bufs(buf_v), writes=_bufs(buf_v))

        S.memset(identF[:], 0.0, "pool"); aff_sel(identF[:], [[-1, T]], ALU.not_equal, 1.0, 1)
        S.cp(identB[:], identF[:], "pool")
        S.memset(onesF[:], 1.0, "pool"); S.memset(mhalf[:], -0.5, "pool")
        S.memset(Um[:], 1.0, "pool"); aff_sel(Um[:], [[1, T]], ALU.is_ge, 0.0, -1)
        S.memset(Vm[:], 1.0, "pool"); aff_sel(Vm[:], [[-1, T]], ALU.is_ge, 0.0, 1)
        S.memset(Lf[:], 1.0, "pool"); aff_sel(Lf[:], [[-1, T]], ALU.is_gt, 0.0, 1)
        S.memset(Lb[:], 1.0, "pool"); aff_sel(Lb[:], [[1, T]], ALU.is_gt, 0.0, -1)
        S.op("pool", lambda e: e.iota(_ap(iota_slot[:]), pattern=[[1, 256]], base=0, channel_multiplier=0,
                                      allow_small_or_imprecise_dtypes=True), writes=[iota_slot])
        S.op("pool", lambda e: e.iota(_ap(iota_part[:]), pattern=[[T, 2]], base=0, channel_multiplier=1,
                                      allow_small_or_imprecise_dtypes=True), writes=[iota_part])

        def mod_gen(st, l, depth=2):
            cT = sb(st, "cT", [T, KT, 3]); sg = sb(st, "sg", [T, KT, 3])
            scT = sb(st, "scT", [T, KT, T])
            awr = ring(st, "aw", [T, KT, 512], F32, depth)
            abr = ring(st, "ab", [3, 512], F32, 2)
            mor = ring(st, "mo", [3, 512], F32, 2)
            pm = ring(st, "pm", [T, 512], F32, 2, psum=True)
            S.dma(cT[:], cT_in[:])
            S.memset(scT[:], 0.0)
            S.act(sg[:], cT[:], AF.Sigmoid)
            S.tt(scT[:, :, 0:3], cT[:], sg[:], ALU.mult)
            for nb in range(12):
                aw = awr.next(); ab = abr.next(); mo = mor.next(); p = pm.next()
                S.dma(aw[:], ada_w[l].rr("(k p) n -> p k n", p=T)[:, :, nb * 512:(nb + 1) * 512])
                S.dma(ab[:], ada_b[l:l + 1, nb * 512:(nb + 1) * 512].bc([3, 512]))
                for k in range(KT):
                    S.mm(p[:], scT[:, k, :], aw[:, k, :], start=(k == 0), stop=(k == KT - 1))
                S.tt(mo[:], p[0:3, :], ab[:], ALU.add)
                S.dma(mod_d[l, :, nb * 512:(nb + 1) * 512], mo[:])
                yield

        with contextlib.ExitStack() as st:
            for _ in mod_gen(st, 0, depth=4):
                pass
        S.barrier()

        def rms_rstd(st_small, ssum, n, rstd_out):
            S.ts(rstd_out, ssum, 1.0 / n, EPS, op0=ALU.mult, op1=ALU.add)
            S.tt(rstd_out, rstd_out, mhalf[:, 0:1], ALU.pow, eng="pool")

        def load_vec_fm(dst, l, v, j):
            S.dma(dst, mod_d[l, v, j * D:(j + 1) * D].rr("(k p) -> p k", p=T), slow=True)

        def load_vec_bc(dst, l, v, j):
            S.dma(dst, mod_d[l, v:v + 1, j * D:(j + 1) * D].bc([T, D]))

        def mixer(l, s, h_src_lat, h_src_ctx, h_dst, last):
            with contextlib.ExitStack() as mx:
                xmT_l = sb(mx, "xmTl", [T, KT, NLAT + 2], BF16)
                xmT_c = sb(mx, "xmTc", [T, KT, NCTX + 2], BF16)
                s1 = sb(mx, "s1", [T, 2, KT]); sh1 = sb(mx, "sh1", [T, 2, KT])
                n1w = sb(mx, "n1w", [T, KT])
                dtraw = sb(mx, "dtraw", [T, 18, 32])
                S.dma(n1w[:], norm1_w[l].rr("(k p) -> p k", p=T), slow=True)
                for i, v in enumerate((s, 2)):
                    load_vec_fm(sh1[:, i, :], l, v, 0)
                    load_vec_fm(s1[:, i, :], l, v, 1)
                S.ts(s1[:], s1[:], 1.0, None, op0=ALU.add)
                S.tt(s1[:], s1[:], n1w[:].us(1).bc([T, 2, KT]), ALU.mult)
                S.memset(xmT_l[:, :, 0:1], 0.0); S.memset(xmT_l[:, :, NLAT + 1:NLAT + 2], 0.0)
                S.memset(xmT_c[:, :, 0:1], 0.0); S.memset(xmT_c[:, :, NCTX + 1:NCTX + 2], 0.0)

                S.phase = f"L{l}s{s}.M1"
                with contextlib.ExitStack() as st:
                    hin = ring(st, "hin", [T, D], F32, 2); xn = ring(st, "xn", [T, D], F32, 2)
                    sq = sb(st, "sq", [T, D]); stat = ring(st, "stat", [T, 2], F32, 2)
                    ptr = ring(st, "ptr", [T, D], F32, 2, psum=True)
                    items = [(h_src_ctx, c, xmT_c, 1) for c in range(2)] + [(h_src_lat, c, xmT_l, 0) for c in range(16)]

                    def m1_s1(it):
                        src, c, xmT, vi = it
                        h_ = hin.next(); x_ = xn.next(); sa = stat.next(); p = ptr.next()
                        S.dma(h_[:], src[c * T:(c + 1) * T, :])
                        S.act(sq[:], h_[:], AF.Square, accum=sa[:, 0:1])
                        rms_rstd(None, sa[:, 0:1], D, sa[:, 1:2])
                        S.ts(x_[:], h_[:], sa[:, 1:2], None, op0=ALU.mult)
                        for k in range(KT):
                            S.tr(p[:, k * T:(k + 1) * T], x_[:, k * T:(k + 1) * T], identF[:])
                        return p

                    def m1_s2(it, p):
                        src, c, xmT, vi = it
                        for k in range(KT):
                            S.act(xmT[:, k, 1 + c * T:1 + (c + 1) * T], p[:, k * T:(k + 1) * T], AF.Identity,
                                  bias=sh1[:, vi, k:k + 1], scale=s1[:, vi, k:k + 1])

                    prev = None
                    for it in items:
                        p_ = m1_s1(it)
                        if prev is not None:
                            m1_s2(*prev)
                        prev = (it, p_)
                    m1_s2(*prev)
                S.barrier()
                if stage == "m1":
                    return

                with contextlib.ExitStack() as sd:
                    xBC_l = sb(sd, "xBCl", [T, 16, NLAT], BF16)
                    xBC_c = sb(sd, "xBCc", [T, 16, NCTX], BF16)
                    Sf = sb(sd, "Sf", [T, D]); Sb_ = sb(sd, "Sb", [T, D])
                    S.phase = f"L{l}s{s}.M2"
                    with contextlib.ExitStack() as st:
                        wx = sb(st, "wx", [T, KT, 2080], BF16)
                        cw = sb(st, "cw", [T, 16, 3]); cb = sb(st, "cb", [T, 16])
                        ctmp = ring(st, "ctmp", [T, 512], F32, 2)
                        pdt = ring(st, "pdt", [T, 32], F32, 2, psum=True)
                        S.dma(wx[:], w_in[l].rr("(k p) n -> p k n", p=T)[:, :, 0:2080], eng="pool")
                        S.dma(cw[:], convw_h[l]); S.dma(cb[:], convb_h[l])
                        mg = mod_gen(st, l + 1) if (s == 0 and l + 1 < nlayers) else None
                        pp = ring(st, "pp", [T, 512], F32, 2 if mg is not None else 4, psum=True)
                        for (nt, xmT, xBC, c0) in ((NCTX, xmT_c, xBC_c, 0), (NLAT, xmT_l, xBC_l, 2)):
                            for c in range(nt // T):
                                p = pdt.next()
                                for k in range(KT):
                                    S.mm(p[:], xmT[:, k, 1 + c * T:1 + (c + 1) * T], wx[:, k, 2048:2080],
                                         start=(k == 0), stop=(k == KT - 1))
                                S.cp(dtraw[:, c0 + c, :], p[:], "act")
                            t0 = 0
                            while t0 < nt:
                                nw = min(510, nt - t0)
                                for q in range(16):
                                    p = pp.next(); tm = ctmp.next()
                                    for k in range(KT):
                                        S.mm(p[:, 0:nw + 2], wx[:, k, q * T:(q + 1) * T], xmT[:, k, t0:t0 + nw + 2],
                                             start=(k == 0), stop=(k == KT - 1))
                                    S.act(tm[:, 0:nw], p[:, 1:nw + 1], AF.Identity, bias=cb[:, q:q + 1], scale=cw[:, q, 1:2])
                                    S.stt(tm[:, 0:nw], p[:, 0:nw], cw[:, q, 0:1], tm[:, 0:nw], ALU.mult, ALU.add)
                                    S.stt(tm[:, 0:nw], p[:, 2:nw + 2], cw[:, q, 2:3], tm[:, 0:nw], ALU.mult, ALU.add)
                                    S.act(xBC[:, q, t0:t0 + nw], tm[:, 0:nw], AF.Silu)
                                    if mg is not None and q % 4 == 3:
                                        try:
                                            next(mg)
                                        except StopIteration:
                                            mg = None
                                t0 += nw
                        if mg is not None:
                            for _ in mg:
                                pass
                    S.barrier()
                    if stage == "m2":
                        dbg["xBC"] = (xBC_l, mx, sd)
                        return

                    S.phase = f"L{l}s{s}.M3"
                    with contextlib.ExitStack() as st:
                        dtb = sb(st, "dtb", [T, 32]); Abc = sb(st, "Abc", [T, 32]); dsk = sb(st, "dsk", [T, 16])
                        xtok = ring(st, "xtok", [T, D], BF16, 2); btok = ring(st, "btok", [T, 512], BF16, 2)
                        dtr = ring(st, "dt", [T, 32], F32, 2); dtAr = ring(st, "dtA", [T, 32], F32, 2)
                        sm = ring(st, "sm", [T, 8, 16], F32, 2)
                        DmR = ring(st, "Dm", [T, 1024], F32, 2); EmR = ring(st, "Em", [T, 1024], F32, 2)
                        MT = ring(st, "MT", [T, 1024], BF16, 4)
                        cbm = ring(st, "cbm", [T, 512], F32, 2)
                        xdt = ring(st, "xdt", [T, D], BF16, 2); xw = ring(st, "xw", [T, D], BF16, 2)
                        Sbf = ring(st, "Sbf", [T, D], BF16, 2)
                        ytmp = ring(st, "ytmp", [T, D], F32, 2); yprev = ring(st, "yprev", [T, D], F32, 2)
                        PA = ps(st, "PA", [T, 1024]); PB = ps(st, "PB", [T, 1024]); PC = ps(st, "PC", [T, 1024])
                        PD = ps(st, "PD", [T, 64]); PT = ps(st, "PT", [T, 1024], BF16)
                        S.dma(dtb[:], dt_bias[l:l + 1, :].bc([T, 32]))
                        S.dma(Abc[:], a_log[l:l + 1, :].bc([T, 32]))
                        S.dma(dsk[:], d_skip[l:l + 1, :].bc([T, 16]))
                        S.act(Abc[:], Abc[:], AF.Exp)
                        S.ts(Abc[:], Abc[:], -1.0, None, op0=ALU.mult)
                        S.memset(Sf[:], 0.0); S.memset(Sb_[:], 0.0)

                        def scan_step(nch, xBC, c0, tok0, direction, need_y, c):
                            fwd = direction == 0
                            St = Sf if fwd else Sb_
                            Lm = Lf if fwd else Lb
                            Cm = Um if fwd else Vm
                            mask = Um if fwd else Vm
                            hs = slice(0, 16) if fwd else slice(16, 32)
                            tsl = slice(c * T, (c + 1) * T)
                            xt = xtok.next(); bt = btok.next(); dt_ = dtr.next(); dA = dtAr.next(); sm_ = sm.next()
                            for k in range(8):
                                S.tr(PT[:, k * T:(k + 1) * T], xBC[:, k, tsl], identB[:])
                            S.cp(xt[:], PT[:], "act")
                            S.tt(dt_[:], dtraw[:, c0 + c, :], dtb[:], ALU.add)
                            S.act(dA[:], dt_[:], AF.Abs)
                            S.act(dA[:], dA[:], AF.Exp, scale=-1.0)
                            S.act(dA[:], dA[:], AF.Ln, bias=1.0)
                            S.ts(dt_[:], dt_[:], 0.0, None, op0=ALU.max)
                            S.tt(dt_[:], dt_[:], dA[:], ALU.add)
                            S.tt(dA[:], dt_[:], Abc[:], ALU.mult)
                            yield
                            for g in range(4):
                                S.tr(PT[:, g * T:(g + 1) * T], xBC[:, 8 + g, tsl], identB[:])
                            S.cp(bt[:], PT[:, 0:512], "act")
                            S.mm(PD[:, 0:16], Cm[:], dA[:, hs])
                            S.mm(PD[:, 16:32], onesF[:], dA[:, hs])
                            S.cp(sm_[:, 0:2, :], PD[:, 0:32].rr("p (a b) -> p a b", a=2))
                            S.act(sm_[:, 2, :], sm_[:, 0, :], AF.Exp)
                            S.tt(sm_[:, 3, :], sm_[:, 1, :], sm_[:, 0, :], ALU.subtract)
                            S.act(sm_[:, 3, :], sm_[:, 3, :], AF.Exp)
                            S.act(sm_[:, 4, :], sm_[:, 1, :], AF.Exp)
                            S.tt(sm_[:, 5, :], sm_[:, 3, :], dt_[:, hs], ALU.mult)
                            xd = xdt.next(); xw_ = xw.next()
                            S.tt(xd[:].rr("p (h q) -> p h q", h=16), xt[:].rr("p (h q) -> p h q", h=16),
                                 dt_[:, hs].us(2).bc([T, 16, 64]), ALU.mult, eng=POOL_ENG)
                            S.tt(xw_[:].rr("p (h q) -> p h q", h=16), xt[:].rr("p (h q) -> p h q", h=16),
                                 sm_[:, 5, :].us(2).bc([T, 16, 64]), ALU.mult, eng=POOL_ENG)
                            yield
                            if need_y:
                                for g in range(4):
                                    S.mm(PC[:, g * T:(g + 1) * T], xBC[:, 8 + g, tsl], xBC[:, 12 + g, tsl])
                                cm_ = cbm.next()
                                S.tt(cm_[:].rr("p (g i) -> p g i", g=4), PC[:, 0:512].rr("p (g i) -> p g i", g=4),
                                     mask[:].us(1).bc([T, 4, T]), ALU.mult)
                                mts = []
                                for half in range(2):
                                    Dm = DmR.next(); Em = EmR.next()
                                    hsel = slice(hs.start + half * 8, hs.start + half * 8 + 8)
                                    if half == 0:
                                        S.tt(Dm[:].rr("p (h i) -> p h i", h=8), Cm[:].us(1).bc([T, 8, T]),
                                             dA[:, hsel].us(2).bc([T, 8, T]), ALU.mult)
                                    else:
                                        for h8 in range(8):
                                            hcol = hs.start + half * 8 + h8
                                            S.act(Dm[:, h8 * T:(h8 + 1) * T], Cm[:], AF.Copy, scale=dA[:, hcol:hcol + 1])
                                    yield
                                    for b2 in range(2):
                                        S.mm(PA[:, b2 * 512:(b2 + 1) * 512], Lm[:], Dm[:, b2 * 512:(b2 + 1) * 512])
                                    S.act(Em[:], PA[:], AF.Exp)
                                    mt = MT.next()
                                    S.tt(mt[:].rr("p (g r i) -> p g r i", g=2, r=4),
                                         Em[:].rr("p (g r i) -> p g r i", g=2, r=4),
                                         cm_[:, half * 256:(half + 1) * 256].rr("p (g i) -> p g i", g=2).us(2).bc([T, 2, 4, T]),
                                         ALU.mult)
                                    mts.append(mt)
                                sbf = Sbf.next()
                                S.cp(sbf[:], St[:], "act")
                                yt = ytmp.next()
                                if fwd:
                                    yp = yprev.next()
                                    S.tt(yp[:].rr("p (h q) -> p h q", h=16), xt[:].rr("p (h q) -> p h q", h=16),
                                         dsk[:].us(2).bc([T, 16, 64]), ALU.mult, eng=POOL_ENG)
                                yield
                                for g in range(4):
                                    S.mm(PC[:, g * 256:(g + 1) * 256], xBC[:, 12 + g, tsl], sbf[:, g * 256:(g + 1) * 256])
                                S.tt(yt[:].rr("p (h q) -> p h q", h=16), PC[:].rr("p (h q) -> p h q", h=16),
                                     sm_[:, 2, :].us(2).bc([T, 16, 64]), ALU.mult)
                                if fwd:
                                    S.tt(yt[:], yt[:], yp[:], ALU.add)
                                yield
                                for h in range(16):
                                    mt = mts[h // 8]; hh_ = h % 8
                                    S.mm(PB[:, h * 64:(h + 1) * 64], mt[:, hh_ * T:(hh_ + 1) * T], xd[:, h * 64:(h + 1) * 64])
                                S.tt(yt[:], yt[:], PB[:], ALU.add)
                                rows = slice(tok0 + c * T, tok0 + (c + 1) * T)
                                S.dma((ybF if fwd else ybT)[s][rows, :], yt[:])
                                yield
                            S.tt(St[:].rr("p (h q) -> p h q", h=16), St[:].rr("p (h q) -> p h q", h=16),
                                 sm_[:, 4, :].us(2).bc([T, 16, 64]), ALU.mult)
                            for g in range(4):
                                S.mm(PC[:, g * 256:(g + 1) * 256], bt[:, g * T:(g + 1) * T], xw_[:, g * 256:(g + 1) * 256])
                            S.tt(St[:], St[:], PC[:], ALU.add)

                        def lockstep(ga, gb):
                            live = [ga, gb]
                            while live:
                                for g_ in list(live):
                                    try:
                                        next(g_)
                                    except StopIteration:
                                        live.remove(g_)

                        for i_ in range(2):
                            lockstep(scan_step(2, xBC_c, 0, 0, 0, not last, i_), scan_step(2, xBC_c, 0, 0, 1, not last, 1 - i_))
                        for i_ in range(16):
                            lockstep(scan_step(16, xBC_l, 2, NCTX, 0, True, i_), scan_step(16, xBC_l, 2, NCTX, 1, True, 15 - i_))
                    S.barrier()
                if stage == "m3":
                    return

                tok_sets = ([(0, 2, xmT_c, 1)] if not last else []) + [(NCTX, 16, xmT_l, 0)]
                items = [(tok0, c, xmT, vi) for (tok0, nch, xmT, vi) in tok_sets for c in range(nch)]

                def pipeline(s1f, s2f):
                    prev = None
                    for it in items + [None]:
                        pieces, cx = s1f(it) if it is not None else ([], None)
                        g = s2f(*prev) if prev is not None else None
                        pieces = list(pieces)
                        while g is not None or pieces:
                            if pieces:
                                pieces.pop(0)()
                            if g is not None:
                                try:
                                    next(g)
                                except StopIteration:
                                    g = None
                        prev = (it, cx)

                S.phase = f"L{l}s{s}.M4a"
                with contextlib.ExitStack() as st:
                    wz = sb(st, "wz", [T, KT, D], BF16); wga = sb(st, "wga", [T, KT, D], BF16); wpa = sb(st, "wpa", [T, KT, D], BF16)
                    snw = sb(st, "snw", [T, D])
                    yin = ring(st, "yin", [T, D], F32, 2); yin2 = ring(st, "yin2", [T, D], F32, 2)
                    zsR = ring(st, "zs", [T, D], F32, 2); gaR = ring(st, "gaT", [T, D], F32, 2)
                    gg = sb(st, "gg", [T, D]); sqs = sb(st, "sqs", [T, 256])
                    gst = ring(st, "gst", [T, 8], F32, 2)
                    stokR = ring(st, "stok", [T, D], BF16, 2); sT = sb(st, "sT", [T, KT, T], BF16)
                    AT = ring(st, "AT", [T, KT, T], F32, 2)
                    PZ = ps(st, "PZ", [T, D]); PT2 = ps(st, "PT2", [T, D], BF16); PPA = ps(st, "PPA", [T, D]); PGA = ps(st, "PGA", [T, D])
                    wv = w_in[l].rr("(k p) n -> p k n", p=T)
                    S.dma(wz[:], wv[:, :, 2080:3104], eng="pool")
                    S.dma(wga[:], wv[:, :, 5152:6176], eng="pool")
                    S.dma(wpa[:], w_pa[l].rr("(k p) n -> p k n", p=T), eng="pool")
                    S.dma(snw[:], ssd_norm_w[l:l + 1, :].bc([T, D]))

                    def a_s1(it):
                        tok0, c, xmT, vi = it
                        rows = slice(tok0 + c * T, tok0 + (c + 1) * T)
                        csl = slice(1 + c * T, 1 + (c + 1) * T)
                        y_ = yin.next(); y2_ = yin2.next(); zs = zsR.next(); gaT = gaR.next()
                        gs = gst.next(); stok = stokR.next()

                        def pz():
                            S.dma(y_[:], ybF[s][rows, :])
                            S.dma(y2_[:], ybT[s][rows, :])
                            for nh in range(2):
                                for k in range(KT):
                                    S.mm(PZ[:, nh * 512:(nh + 1) * 512], xmT[:, k, csl], wz[:, k, nh * 512:(nh + 1) * 512],
                                         start=(k == 0), stop=(k == KT - 1))
                            S.act(zs[:], PZ[:], AF.Silu)
                            S.tt(y_[:], y_[:], y2_[:], ALU.add)
                            S.tt(gg[:], y_[:], zs[:], ALU.mult)
                            for g in range(4):
                                S.act(sqs[:], gg[:, g * 256:(g + 1) * 256], AF.Square, accum=gs[:, g:g + 1])
                            S.ts(gs[:, 4:8], gs[:, 0:4], 1.0 / 256, EPS, op0=ALU.mult, op1=ALU.add)
                            S.tt(gs[:, 4:8], gs[:, 4:8], mhalf[:, 0:4], ALU.pow, eng="pool")
                            S.tt(gg[:].rr("p (g q) -> p g q", g=4), gg[:].rr("p (g q) -> p g q", g=4),
                                 gs[:, 4:8].us(2).bc([T, 4, 256]), ALU.mult)
                            S.tt(stok[:], gg[:], snw[:], ALU.mult)

                        def pga():
                            for dt_ in range(KT):
                                for k in range(KT):
                                    S.mm(PGA[:, dt_ * T:(dt_ + 1) * T], wga[:, k, dt_ * T:(dt_ + 1) * T], xmT[:, k, csl],
                                         start=(k == 0), stop=(k == KT - 1))
                            S.act(gaT[:], PGA[:], AF.Tanh, scale=0.5)
                        return [pz, pga], (stok, gaT)

                    def a_s2(it, cx):
                        tok0, c, xmT, vi = it
                        stok, gaT = cx
                        rows = slice(tok0 + c * T, tok0 + (c + 1) * T)
                        at = AT.next()
                        for k in range(KT):
                            S.tr(PT2[:, k * T:(k + 1) * T], stok[:, k * T:(k + 1) * T], identB[:])
                        S.cp(sT[:].rr("p k t -> p (k t)"), PT2[:], "act")
                        yield
                        for dt_ in range(KT):
                            for k in range(KT):
                                S.mm(PPA[:, dt_ * T:(dt_ + 1) * T], wpa[:, k, dt_ * T:(dt_ + 1) * T], sT[:, k, :],
                                     start=(k == 0), stop=(k == KT - 1))
                        S.stt(at[:].rr("p k t -> p (k t)"), gaT[:], 1.0, PPA[:], ALU.add, ALU.mult)
                        S.dma(atb[s][(tok0 // T) + c], at[:].rr("p k t -> p (k t)"), eng="pool")

                    pipeline(a_s1, a_s2)
                S.barrier()
                if stage == "m4a":
                    return

                S.phase = f"L{l}s{s}.M4b"
                with contextlib.ExitStack() as st:
                    wu = sb(st, "wu", [T, KT, D], BF16); wvv = sb(st, "wvv", [T, KT, D], BF16); wgb = sb(st, "wgb", [T, KT, D], BF16)
                    wpb = sb(st, "wpb", [T, KT, D], BF16); wo = sb(st, "wo", [T, KT, D], BF16)
                    lnw = sb(st, "lnw", [T, D]); lnb = sb(st, "lnb", [T, D]); bsb = sb(st, "bsb", [T, D]); g1b = sb(st, "g1b", [T, 2, D])
                    wsT = sb(st, "wsT", [T, 8, T], BF16)
                    Q0 = ps(st, "Q0", [T, D]); Q2 = ps(st, "Q2", [T, D]); QG = ps(st, "QG", [T, D]); QS = ps(st, "QS", [T, D])
                    wv = w_in[l].rr("(k p) n -> p k n", p=T)
                    S.dma(wvv[:], wv[:, :, 4128:5152], eng="pool")
                    S.dma(wu[:], wv[:, :, 3104:4128], eng="pool")
                    S.dma(wgb[:], wv[:, :, 6176:7200], eng="pool")
                    S.dma(wpb[:], w_pb[l].rr("(k p) n -> p k n", p=T), eng="pool")
                    S.dma(wo[:], w_o[l].rr("(k p) n -> p k n", p=T), eng="pool")
                    S.dma(lnw[:], sgu_ln_w[l:l + 1, :].bc([T, D]))
                    S.dma(lnb[:], sgu_ln_b[l:l + 1, :].bc([T, D]))
                    S.dma(bsb[:], b_s[l:l + 1, :].bc([T, D]))
                    for i, v in enumerate((s, 2)):
                        load_vec_bc(g1b[:, i, :], l, v, 2)
                    S.ts(g1b[:], g1b[:], 0.5, None, op0=ALU.mult)
                    with contextlib.ExitStack() as st2:
                        wsl = sb(st2, "wsl", [T, 8, T])
                        S.dma(wsl[:], w_s[l].rr("g i j -> i g j"))
                        for g in range(8):
                            S.tr(Q0[:, g * T:(g + 1) * T], wsl[:, g, :], identF[:])
                        S.cp(wsT[:].rr("p g i -> p (g i)"), Q0[:])
                    S.barrier()
                    vvR = ring(st, "vv", [T, D], F32, 1); uTR = ring(st, "uT", [T, D], F32, 2); gbR = ring(st, "gbT", [T, D], F32, 2)
                    vnR = ring(st, "vn", [T, D], BF16, 2); tmpv = sb(st, "tmpv", [T, D])
                    sgT = sb(st, "sgT", [T, KT, T], BF16); mT = sb(st, "mT", [T, KT, T], BF16)
                    bst = ring(st, "bst", [T, 16], F32, 2)
                    ATi = ring(st, "ATi", [T, KT, T], F32, 2)
                    hin = ring(st, "hin2", [T, D], F32, 2)

                    def b_s1(it):
                        tok0, c, xmT, vi = it
                        rows = slice(tok0 + c * T, tok0 + (c + 1) * T)
                        csl = slice(1 + c * T, 1 + (c + 1) * T)
                        ati = ATi.next(); h_ = hin.next(); vv = vvR.next(); uT = uTR.next(); gbT = gbR.next()
                        vn = vnR.next(); bs_ = bst.next()
                        src = (h_src_ctx if tok0 == 0 else h_src_lat)

                        def pv():
                            S.dma(ati[:].rr("p k t -> p (k t)"), atb[s][(tok0 // T) + c])
                            S.dma(h_[:], src[c * T:(c + 1) * T, :])
                            for nh in range(2):
                                for k in range(KT):
                                    S.mm(Q0[:, nh * 512:(nh + 1) * 512], xmT[:, k, csl], wvv[:, k, nh * 512:(nh + 1) * 512],
                                         start=(k == 0), stop=(k == KT - 1))
                            S.act(vv[:], Q0[:], AF.Gelu)

                        def pln():
                            for i2 in range(2):
                                S.op("dve", lambda e, i2=i2: e.bn_stats(out=_ap(bs_[:, i2 * 6:(i2 + 1) * 6]),
                                                                        in_=_ap(vv[:, i2 * 512:(i2 + 1) * 512])),
                                     reads=[vv], writes=[bs_])
                            S.op("dve", lambda e: e.bn_aggr(out=_ap(bs_[:, 12:14]), in_=_ap(bs_[:, 0:12])),
                                 reads=[bs_], writes=[bs_])
                            S.ts(bs_[:, 14:15], bs_[:, 13:14], 1.0, EPS, op0=ALU.mult, op1=ALU.add)
                            S.tt(bs_[:, 14:15], bs_[:, 14:15], mhalf[:, 0:1], ALU.pow, eng="pool")
                            S.ts(vv[:], vv[:], bs_[:, 12:13], bs_[:, 14:15], op0=ALU.subtract, op1=ALU.mult)
                            S.tt(vv[:], vv[:], lnw[:], ALU.mult)
                            S.tt(vn[:], vv[:], lnb[:], ALU.add)

                        def pu():
                            for dt_ in range(KT):
                                for k in range(KT):
                                    S.mm(Q2[:, dt_ * T:(dt_ + 1) * T], wu[:, k, dt_ * T:(dt_ + 1) * T], xmT[:, k, csl],
                                         start=(k == 0), stop=(k == KT - 1))
                            S.act(uT[:], Q2[:], AF.Gelu)

                        def pgb():
                            for dt_ in range(KT):
                                for k in range(KT):
                                    S.mm(QG[:, dt_ * T:(dt_ + 1) * T], wgb[:, k, dt_ * T:(dt_ + 1) * T], xmT[:, k, csl],
                                         start=(k == 0), stop=(k == KT - 1))
                            S.act(gbT[:], QG[:], AF.Tanh, scale=0.5)
                        return [pv, pu, pgb, pln], (ati, h_, vn, uT, gbT)

                    def b_s2(it, cx):
                        tok0, c, xmT, vi = it
                        ati, h_, vn, uT, gbT = cx
                        vv = tmpv
                        rows = slice(tok0 + c * T, tok0 + (c + 1) * T)
                        for g in range(8):
                            S.mm(QS[:, g * T:(g + 1) * T], vn[:, g * T:(g + 1) * T], wsT[:, g, :])
                        yield
                        S.tt(vv[:], QS[:], bsb[:], ALU.add)
                        S.tt(sgT[:].rr("p k t -> p (k t)"), uT[:], vv[:], ALU.mult)
                        for dt_ in range(KT):
                            for k in range(KT):
                                S.mm(QS[:, dt_ * T:(dt_ + 1) * T], wpb[:, k, dt_ * T:(dt_ + 1) * T], sgT[:, k, :],
                                     start=(k == 0), stop=(k == KT - 1))
                        yield
                        S.stt(gbT[:], gbT[:], 1.0, QS[:], ALU.add, ALU.mult)
                        S.tt(mT[:].rr("p k t -> p (k t)"), gbT[:], ati[:].rr("p k t -> p (k t)"), ALU.add)
                        for nh in range(2):
                            for k in range(KT):
                                S.mm(QS[:, nh * 512:(nh + 1) * 512], mT[:, k, :], wo[:, k, nh * 512:(nh + 1) * 512],
                                     start=(k == 0), stop=(k == KT - 1))
                        yield
                        S.tt(vv[:], QS[:], g1b[:, vi, :], ALU.mult)
                        S.tt(h_[:], h_[:], vv[:], ALU.add)
                        S.dma(h_dst[rows, :], h_[:], eng="pool")

                    pipeline(b_s1, b_s2)
                S.barrier()

        def moe(l, s, sets, h_src):
            with contextlib.ExitStack() as mo:
                for ts_ in sets:
                    nch = ts_["nch"]; n = nch * T
                    ts_["CT"] = (ts_["cap"] + T - 1) // T
                    ts_["CP"] = min(ts_["cap"], T)
                    ts_["xf"] = sb(mo, "xf", [T, nch, D], BF16)
                    ts_["affhl"] = sb(mo, "affhl", [T, nch, NE, 2], BF16)
                    ts_["posm_tok"] = sb(mo, "posmtok", [T, nch, NE])
                    ts_["posmT"] = sb(mo, "posmT", [T, n], BF16)
                wst = contextlib.ExitStack()
                wr = ring(wst, "wexp", [T, KT, D], BF16, 4)
                pre_w = []
                for wsrc in (w_gate, w_up, w_down):
                    w_ = wr.next(); S.dma(w_[:], wsrc[l, 0].rr("(k p) f -> p k f", p=T), eng="pool"); pre_w.append(w_)
                S.phase = f"L{l}s{s}.moeR"
                for ts_ in sets:
                    nch = ts_["nch"]; n = nch * T; v = ts_["v"]; tok0 = ts_["tok0"]; cap = ts_["cap"]
                    xf = ts_["xf"]; affhl = ts_["affhl"]; posm_tok = ts_["posm_tok"]; posmT = ts_["posmT"]
                    with contextlib.ExitStack() as st:
                        s2b = sb(st, "s2b", [T, D]); sh2b = sb(st, "sh2b", [T, D]); n2w = sb(st, "n2w", [T, D])
                        rw = sb(st, "rw", [T, KT, NE])
                        hin = ring(st, "hin3", [T, D], F32, 2); xff = ring(st, "xff", [T, D], F32, 2)
                        sq = sb(st, "sq3", [T, D]); stat = ring(st, "stat3", [T, 2], F32, 2)
                        xfT = ring(st, "xfT", [T, KT, T], F32, 2)
                        aff = sb(st, "aff", [T, nch, NE]); lg = sb(st, "lg", [T, nch, NE]); sm = sb(st, "smx", [T, 2, nch])
                        affT = sb(st, "affT", [NE, n]); maskT = sb(st, "maskT", [NE, n]); inclT = sb(st, "inclT", [NE, n])
                        onesT = sb(st, "onesT", [NE, n]); pmf = sb(st, "pmf", [NE, n])
                        bis = sb(st, "bis", [NE, 4])
                        ptr = ring(st, "ptr3", [T, D], F32, 2, psum=True)
                        plg = ps(st, "plg", [T, nch, NE]); pat = ps(st, "pat", [NE, 512]); ppt = ps(st, "ppt", [T, 512])
                        load_vec_bc(sh2b[:], l, v, 3)
                        load_vec_bc(s2b[:], l, v, 4)
                        S.dma(n2w[:], norm2_w[l:l + 1, :].bc([T, D]))
                        S.dma(rw[:], router_w[l].rr("(k p) e -> p k e", p=T))
                        S.ts(s2b[:], s2b[:], 1.0, None, op0=ALU.add)
                        S.tt(s2b[:], s2b[:], n2w[:], ALU.mult)

                        def r_s1(c):
                            h_ = hin.next(); x_ = xff.next(); sa = stat.next(); p = ptr.next()
                            S.dma(h_[:], h_src[tok0 + c * T:tok0 + (c + 1) * T, :])
                            S.act(sq[:], h_[:], AF.Square, accum=sa[:, 0:1])
                            rms_rstd(None, sa[:, 0:1], D, sa[:, 1:2])
                            S.stt(x_[:], h_[:], sa[:, 1:2], s2b[:], ALU.mult, ALU.mult)
                            S.tt(x_[:], x_[:], sh2b[:], ALU.add)
                            S.cp(xf[:, c, :], x_[:], "act")
                            for k in range(KT):
                                S.tr(p[:, k * T:(k + 1) * T], x_[:, k * T:(k + 1) * T], identF[:])
                            return p

                        def r_s2(c, p):
                            xt_ = xfT.next()
                            S.cp(xt_[:].rr("p k t -> p (k t)"), p[:], "act")
                            for k in range(KT):
                                S.mm(plg[:, c, :], xt_[:, k, :], rw[:, k, :], start=(k == 0), stop=(k == KT - 1))

                        prev = None
                        for c in range(nch):
                            p_ = r_s1(c)
                            if prev is not None:
                                r_s2(*prev)
                            prev = (c, p_)
                        r_s2(*prev)
                        S.cp(lg[:], plg[:])
                        S.red(sm[:, 0, :], lg[:], ALU.max)
                        S.tt(lg[:], lg[:], sm[:, 0, :].us(2).bc([T, nch, NE]), ALU.subtract)
                        S.act(lg[:], lg[:], AF.Exp)
                        S.red(sm[:, 1, :], lg[:], ALU.add)
                        S.recip(sm[:, 1, :], sm[:, 1, :])
                        S.tt(aff[:], lg[:], sm[:, 1, :].us(2).bc([T, nch, NE]), ALU.mult)
                        S.cp(affhl[:, :, :, 0], aff[:])
                        S.tt(lg[:], aff[:], affhl[:, :, :, 0], ALU.subtract)
                        S.cp(affhl[:, :, :, 1], lg[:])
                        for c in range(nch):
                            off = (c % 4) * T
                            S.tr(pat[:, off:off + T], aff[:, c, :], identF[:])
                            if c % 4 == 3 or c == nch - 1:
                                w_ = off + T
                                S.cp(affT[:, (c // 4) * 512:(c // 4) * 512 + w_], pat[:, 0:w_])
                        S.memset(bis[:], 0.0)
                        S.memset(onesT[:], 1.0)
                        for it in range(NBIS):
                            half = 2.0 ** (-(it + 1))
                            S.ts(bis[:, 1:2], bis[:, 0:1], half, None, op0=ALU.add)
                            S.ts(maskT[:], affT[:], bis[:, 1:2], None, op0=ALU.is_ge, op1=ALU.add, accum=bis[:, 2:3])
                            S.ts(bis[:, 3:4], bis[:, 2:3], float(cap), half, op0=ALU.is_ge, op1=ALU.mult)
                            S.tt(bis[:, 0:1], bis[:, 0:1], bis[:, 3:4], ALU.add)
                        S.ts(maskT[:], affT[:], bis[:, 0:1], None, op0=ALU.is_ge)
                        S.op("dve", lambda e, inclT=inclT, onesT=onesT, maskT=maskT: e.tensor_tensor_scan(
                            out=_ap(inclT[:]), data0=_ap(onesT[:]), data1=_ap(maskT[:]), initial=0.0, op0=ALU.mult, op1=ALU.add),
                             reads=[onesT, maskT], writes=[inclT])
                        S.tt(pmf[:], inclT[:], maskT[:], ALU.mult)
                        S.ts(pmf[:], pmf[:], -1.0, None, op0=ALU.add)
                        S.memset(posmT[:], 0.0)
                        S.cp(posmT[0:NE, :], pmf[:])
                        for c in range(nch):
                            off = (c % 4) * NE
                            S.tr(ppt[:, off:off + NE], pmf[:, c * T:(c + 1) * T], identF[0:NE, 0:NE])
                            if c % 4 == 3 or c == nch - 1:
                                w_ = off + NE
                                c0_ = (c // 4) * 4
                                S.cp(posm_tok[:, c0_:c + 1, :].rr("p c e -> p (c e)"), ppt[:, 0:w_])
                    S.barrier()

                S.phase = f"L{l}s{s}.moeE"
                with contextlib.ExitStack() as st:
                    mcap = max(t_["cap"] for t_ in sets); mnch = max(t_["nch"] for t_ in sets)
                    Sg = ring(st, "Sg", [T, mnch, mcap], BF16, 2)
                    xgT = ring(st, "xgT", [T, KT, mcap], BF16, 2)
                    hT = ring(st, "hT", [T, KT, mcap], BF16, 2)
                    sil = ring(st, "sil", [T, mcap], F32, 2)
                    gsl = ring(st, "gsl", [T, 4], F32, 2)
                    yw = ring(st, "yw", [T, 2, D], BF16, 2)
                    pg = ring(st, "pg", [T, 512], F32, 2, psum=True)
                    pf = ring(st, "pf", [T, 512], F32, 2, psum=True)
                    pd = ring(st, "pd", [T, 512], F32, 2, psum=True)
                    pgs = ps(st, "pgs", [T, 4])
                    for e_ in range(NE):
                        if e_ == 0:
                            wg_, wu_, wd_ = pre_w
                        else:
                            wg_ = wr.next(); S.dma(wg_[:], w_gate[l, e_].rr("(k p) f -> p k f", p=T), eng="pool")
                            wu_ = wr.next(); S.dma(wu_[:], w_up[l, e_].rr("(k p) f -> p k f", p=T), eng="pool")
                            wd_ = wr.next(); S.dma(wd_[:], w_down[l, e_].rr("(k p) f -> p k f", p=T), eng="pool")
                        def eset(ts_, e_, wg_, wu_, wd_):
                            nch = ts_["nch"]; cap = ts_["cap"]; CT = ts_["CT"]; CP = ts_["CP"]
                            xf = ts_["xf"]; affhl = ts_["affhl"]; posm_tok = ts_["posm_tok"]
                            sg_ = Sg.next(); xg = xgT.next(); ht = hT.next(); gs = gsl.next(); yw_ = yw.next()
                            for c in range(nch):
                                S.ts(sg_[:, c, 0:cap], iota_slot[:, 0:cap], posm_tok[:, c, e_:e_ + 1], None, op0=ALU.is_equal)
                            yield
                            for ct in range(CT):
                                for c in range(nch):
                                    S.mm(pgs[0:CP, ct * 2:ct * 2 + 2], sg_[:, c, ct * T:ct * T + CP], affhl[:, c, e_, :],
                                         start=(c == 0), stop=(c == nch - 1))
                            S.red(gs[0:CP, 0:CT], pgs[0:CP, 0:2 * CT].rr("p (a b) -> p a b", b=2), ALU.add)
                            p = None
                            for k in range(KT):
                                p = pg.next() if k % 2 == 0 else p
                                o_ = (k % 2) * 256
                                for c in range(nch):
                                    S.mm(p[:, o_:o_ + cap], xf[:, c, k * T:(k + 1) * T], sg_[:, c, 0:cap],
                                         start=(c == 0), stop=(c == nch - 1))
                                S.cp(xg[:, k, 0:cap], p[:, o_:o_ + cap], "act")
                                if k % 4 == 3:
                                    yield
                            for f in range(KT):
                                p = pf.next(); sl = sil.next()
                                for k in range(KT):
                                    S.mm(p[:, 0:cap], wg_[:, k, f * T:(f + 1) * T], xg[:, k, 0:cap], start=(k == 0), stop=(k == KT - 1))
                                for k in range(KT):
                                    S.mm(p[:, 256:256 + cap], wu_[:, k, f * T:(f + 1) * T], xg[:, k, 0:cap], start=(k == 0), stop=(k == KT - 1))
                                S.act(sl[:, 0:cap], p[:, 0:cap], AF.Silu)
                                S.tt(ht[:, f, 0:cap], sl[:, 0:cap], p[:, 256:256 + cap], ALU.mult)
                                if f % 4 == 3:
                                    yield
                            for ct in range(CT):
                                for dh in range(2):
                                    p = pd.next()
                                    for f in range(KT):
                                        S.mm(p[0:CP, :], ht[:, f, ct * T:ct * T + CP], wd_[:, f, dh * 512:(dh + 1) * 512],
                                             start=(f == 0), stop=(f == KT - 1))
                                    S.act(yw_[0:CP, ct, dh * 512:(dh + 1) * 512], p[0:CP, :], AF.Copy, scale=gs[0:CP, ct:ct + 1])
                            S.dma(ts_["yw"][e_, 0:CT, 0:CP, :].rr("c p d -> p c d"), yw_[0:CP, 0:CT, :])

                        live = [eset(ts_, e_, wg_, wu_, wd_) for ts_ in sets]
                        while live:
                            for g_ in list(live):
                                try:
                                    next(g_)
                                except StopIteration:
                                    live.remove(g_)
                S.barrier()
                wst.close()

                S.phase = f"L{l}s{s}.moeB"
                for ts_ in sets:
                    nch = ts_["nch"]; cap = ts_["cap"]; CT = ts_["CT"]; CP = ts_["CP"]; v = ts_["v"]; tok0 = ts_["tok0"]
                    posmT = ts_["posmT"]; final = ts_["final"]; h_dst_v = ts_["dst"]
                    with contextlib.ExitStack() as st:
                        ywa = sb(st, "ywa", [T, NE, CT, D], BF16)
                        onehotE = sb(st, "onehotE", [T, NE, T], BF16)
                        S.memset(onehotE[:], 0.0, "pool")
                        aff_sel(onehotE[:], [[-1, NE], [0, T]], ALU.not_equal, 1.0, 1)
                        STt = ring(st, "STt", [T, CT, NE * T], BF16, 2)
                        g2b = sb(st, "g2b", [T, D]); fnw = sb(st, "fnw", [T, D])
                        hin = ring(st, "hin4", [T, D], F32, 2); ho = ring(st, "ho4", [T, D], F32, 2)
                        sq = sb(st, "sq4", [T, D]); stat = ring(st, "stat4", [T, 2], F32, 2)
                        ppb = ps(st, "ppb", [T, NE * T]); po = ps(st, "po", [T, D])
                        load_vec_bc(g2b[:], l, v, 5)
                        if final:
                            S.dma(fnw[:], final_norm_w[0:1, :].bc([T, D]))
                        S.dma(ywa[0:CP, :, :, :], ts_["yw"][:, 0:CT, 0:CP, :].rr("e c p d -> p e c d"))

                        def b_s1(c):
                            stt_ = STt.next(); h_ = hin.next()
                            S.dma(h_[:], h_src[tok0 + c * T:tok0 + (c + 1) * T, :])
                            for e_ in range(NE):
                                S.mm(ppb[:, e_ * T:(e_ + 1) * T], onehotE[:, e_, :], posmT[:, c * T:(c + 1) * T])
                            for ct in range(CT):
                                S.ts(stt_[0:CP, ct, :], ppb[0:CP, :], iota_part[0:CP, ct:ct + 1], None, op0=ALU.is_equal)
                            return (stt_, h_)

                        def b_s2(c, cx):
                            stt_, h_ = cx
                            o_ = ho.next(); sa = stat.next()
                            for dh in range(2):
                                i_ = 0
                                for e_ in range(NE):
                                    for ct in range(CT):
                                        S.mm(po[:, dh * 512:(dh + 1) * 512], stt_[0:CP, ct, e_ * T:(e_ + 1) * T],
                                             ywa[0:CP, e_, ct, dh * 512:(dh + 1) * 512],
                                             start=(i_ == 0), stop=(i_ == NE * CT - 1))
                                        i_ += 1
                            S.tt(o_[:], po[:], g2b[:], ALU.mult)
                            S.tt(o_[:], o_[:], h_[:], ALU.add)
                            if final:
                                S.act(sq[:], o_[:], AF.Square, accum=sa[:, 0:1])
                                rms_rstd(None, sa[:, 0:1], D, sa[:, 1:2])
                                S.stt(o_[:], o_[:], sa[:, 1:2], fnw[:], ALU.mult, ALU.mult)
                            S.dma(h_dst_v[c * T:(c + 1) * T, :], o_[:])

                        prev = None
                        for c in range(nch):
                            cx = b_s1(c)
                            if prev is not None:
                                b_s2(*prev)
                            prev = (c, cx)
                        b_s2(*prev)
                    S.barrier()

        for l in range(nlayers):
            last = (l == DEPTH - 1)
            for s in range(nseq):
                if l == 0:
                    src_l, src_c = x_in[s], ctx_in[s]
                else:
                    src_l, src_c = hB[s][NCTX:NTOK, :], hB[s][0:NCTX, :]
                mixer(l, s, src_l, src_c, hA[s], last)
                if stage in ("m1", "m2", "m3", "m4a"):
                    continue
                if stage == "mix":
                    continue
                if not last:
                    sets = [dict(v=2, tok0=0, nch=2, cap=32, dst=hB[s][0:NCTX, :], final=False, yw=ywc[s]),
                            dict(v=s, tok0=NCTX, nch=16, cap=256, dst=hB[s][NCTX:NTOK, :], final=False, yw=ywb[s])]
                else:
                    sets = [dict(v=s, tok0=NCTX, nch=16, cap=256, dst=y_out[s], final=True, yw=ywb[s])]
                moe(l, s, sets, hA[s])

        outs = [y_out]
        if stage != "full":
            with contextlib.ExitStack() as st:
                tmp = ring(st, "dbgt", [T, D], F32, 2)
                for s in range(nseq):
                    srcs = {"m3": ybT[s], "mix": hA[s], "m4a": None, "m1": None, "m2": None, "moe0": hB[s]}.get(stage)
                    if srcs is None:
                        continue
                    for c in range(16):
                        t_ = tmp.next()
                        S.dma(t_[:], srcs[NCTX + c * T:NCTX + (c + 1) * T, :])
                        S.dma(y_out[s, c * T:(c + 1) * T, :], t_[:])
        S.emit(final_waits=[y_out])
    return nc


def _prep_common(inp):
    L = DEPTH
    f = lambda a: np.ascontiguousarray(np.asarray(a, dtype=np.float32))
    com = {
        "ada_w": f(inp["ada_w"]), "ada_b": f(inp["ada_b"]),
        "norm1_w": f(inp["norm1_w"]), "norm2_w": f(inp["norm2_w"]),
        "w_in": f(inp["w_in"]),
        "convw_h": f(np.asarray(inp["conv_w"]).reshape(L, 3, 16, T).transpose(0, 3, 2, 1)),
        "convb_h": f(np.asarray(inp["conv_b"]).reshape(L, 16, T).transpose(0, 2, 1)),
        "dt_bias": f(np.asarray(inp["dt_bias"]).reshape(L, 32)),
        "a_log": f(np.asarray(inp["a_log"]).reshape(L, 32)),
        "d_skip": f(inp["d_skip"]),
        "ssd_norm_w": f(inp["ssd_norm_w"]), "sgu_ln_w": f(inp["sgu_ln_w"]), "sgu_ln_b": f(inp["sgu_ln_b"]),
        "w_s": f(inp["w_s"]), "b_s": f(np.asarray(inp["b_s"]).reshape(L, 8 * T)),
        "w_pa": f(inp["w_pa"]), "w_pb": f(inp["w_pb"]), "w_o": f(inp["w_o"]),
        "router_w": f(inp["router_w"]),
        "w_gate": f(inp["w_gate"]), "w_up": f(inp["w_up"]), "w_down": f(inp["w_down"]),
        "final_norm_w": f(np.asarray(inp["final_norm_w"]).reshape(1, D)),
    }
    return com


def _core_inputs(inp, com, seqs):
    x = np.asarray(inp["x"], dtype=np.float32); ctx = np.asarray(inp["ctx"], dtype=np.float32)
    c = np.asarray(inp["c"], dtype=np.float32); c_ctx = np.asarray(inp["c_ctx"], dtype=np.float32)
    vecs = [c[b] for b in seqs]
    while len(vecs) < 2:
        vecs.append(c[seqs[0]])
    cv = np.stack(vecs + [c_ctx], axis=0)
    cT = np.ascontiguousarray(cv.reshape(3, KT, T).transpose(2, 1, 0))
    m = dict(com)
    m["x"] = np.ascontiguousarray(x[list(seqs)])
    m["ctx"] = np.ascontiguousarray(ctx[list(seqs)])
    m["cT"] = cT
    return m


def kernel(**inputs):
    nseq = 2
    nc = build_nc(nseq=nseq, stage="full")
    com = _prep_common(inputs)
    in_maps = [_core_inputs(inputs, com, (2 * i, 2 * i + 1)) for i in range(NCORES)]
    res = run_bass_kernel_spmd(nc, in_maps, core_ids=list(range(NCORES)))
    out = np.concatenate([np.asarray(r["y"], dtype=np.float32) for r in res.results], axis=0)
    return out
```

```python
import contextlib
import numpy as np
import concourse.bass as bass
import concourse.mybir as mybir
from concourse.bass_utils import run_bass_kernel_spmd

F32 = mybir.dt.float32
BF16 = mybir.dt.bfloat16
AF = mybir.ActivationFunctionType
ALU = mybir.AluOpType
AX = mybir.AxisListType.X

NCORES = 8
D = 1024
KT = 8
T = 128
NLAT = 2048
NCTX = 256
NTOK = NLAT + NCTX
DEPTH = 2
NIN = 7200
NE = 16
EPS = 1e-6
NBIS = 26
POOL_ENG = "dve"
import os
PHASEMAP = os.environ.get("MK_PHASEMAP", "")


class Buf:
    def __init__(self, t, name):
        self.t = t
        self.name = name
        self.last_writer = None
        self.readers = []
        self.sem = None
        self.dma_count = 0
        self.nowaw = False

    def __getitem__(self, idx):
        return V(self, self.t[idx])


class V:
    def __init__(self, buf, ap):
        self.buf = buf
        self.ap = ap

    def __getitem__(self, idx):
        return V(self.buf, self.ap[idx])

    def rr(self, s, **kw):
        return V(self.buf, self.ap.rearrange(s, **kw))

    def bc(self, shape):
        return V(self.buf, self.ap.broadcast_to(list(shape)))

    def us(self, axis):
        return V(self.buf, self.ap.unsqueeze(axis))


def _ap(x):
    return x.ap if isinstance(x, V) else x


def _bufs(*xs):
    out = []
    for x in xs:
        if isinstance(x, V) and x.buf not in out:
            out.append(x.buf)
    return out


class Op:
    __slots__ = ("eng", "fn", "is_dma", "waits", "signal", "count", "dst", "idx", "slot", "epoch", "phase")


class Sched:
    ENG = ("pe", "act", "dve", "pool", "sp")

    def __init__(self, nc):
        self.nc = nc
        self.ops = []
        self.per_eng = {e: [] for e in self.ENG}
        self.pending_barrier = {e: None for e in self.ENG}
        self.epoch = 0
        self.slot_count = []
        self.epoch_slots = {}
        self.phase = "init"
        self.phase_map = {}

    def _slot(self, buf):
        if buf not in self.epoch_slots:
            i = len(self.epoch_slots)
            if i >= len(self.slot_count):
                self.slot_count.append(0)
            self.epoch_slots[buf] = i
        return self.epoch_slots[buf]

    def barrier(self):
        last = {e: (self.per_eng[e][-1] if self.per_eng[e] else None) for e in self.ENG}
        dmas = list(self.slot_count)
        for e in self.ENG:
            self.pending_barrier[e] = (last, dmas)
        self.epoch += 1
        self.epoch_slots = {}

    def op(self, eng, fn, reads=(), writes=(), dma=False):
        o = Op()
        o.eng = eng; o.fn = fn; o.is_dma = dma; o.waits = []; o.signal = False
        o.count = None; o.idx = len(self.ops); o.dst = None; o.slot = None; o.epoch = self.epoch
        o.phase = self.phase
        deps = []
        for b in reads:
            if b.last_writer is not None:
                deps.append((b.last_writer, "raw"))
        for b in writes:
            if b.last_writer is not None and not (dma and b.nowaw and b.last_writer.is_dma):
                deps.append((b.last_writer, "waw"))
            for r in b.readers:
                deps.append((r, "war"))
        if dma:
            assert len(writes) == 1
            o.dst = writes[0]
            o.slot = self._slot(o.dst)
            self.slot_count[o.slot] += 1
        pb = self.pending_barrier[eng]
        if pb is not None:
            last, dmas = pb
            for e2, lo in last.items():
                if lo is not None and not lo.is_dma:
                    if e2 == eng and not dma:
                        continue
                    lo.signal = True
                    o.waits.append(("eng", lo))
            for i, cnt in enumerate(dmas):
                if cnt > 0:
                    o.waits.append(("dma", i, cnt))
            self.pending_barrier[eng] = None
        for (d, kind) in deps:
            if d is o:
                continue
            if d.is_dma:
                if d.epoch != self.epoch:
                    continue
                cnt = self.slot_count[d.slot] - (1 if (dma and d.slot == o.slot) else 0)
                o.waits.append(("dma", d.slot, cnt))
            else:
                if d.eng == eng and not dma:
                    if eng == "pe":
                        continue
                    if kind != "raw":
                        continue
                d.signal = True
                o.waits.append(("eng", d))
        for b in writes:
            b.last_writer = o
            b.readers = []
        for b in reads:
            if b not in writes:
                b.readers.append(o)
        self.ops.append(o)
        self.per_eng[eng].append(o)
        return o

    def mm(self, out, lhsT, rhs, start=True, stop=True):
        self.op("pe", lambda e: e.matmul(out=_ap(out), lhsT=_ap(lhsT), rhs=_ap(rhs), start=start, stop=stop),
                reads=_bufs(lhsT, rhs), writes=_bufs(out))

    def tr(self, out, in_, ident):
        self.op("pe", lambda e: e.transpose(out=_ap(out), in_=_ap(in_), identity=_ap(ident)),
                reads=_bufs(in_, ident), writes=_bufs(out))

    def act(self, out, in_, func, bias=0.0, scale=1.0, accum=None):
        kw = {}
        if accum is not None:
            kw["accum_out"] = _ap(accum)
        self.op("act", lambda e: e.activation(out=_ap(out), in_=_ap(in_), func=func, bias=_ap(bias), scale=_ap(scale), **kw),
                reads=_bufs(in_, bias, scale), writes=_bufs(out, accum))

    def tt(self, out, in0, in1, op, eng="dve"):
        self.op(eng, lambda e: e.tensor_tensor(out=_ap(out), in0=_ap(in0), in1=_ap(in1), op=op),
                reads=_bufs(in0, in1), writes=_bufs(out))

    def ts(self, out, in0, s1, s2=None, op0=ALU.mult, op1=None, accum=None, eng="dve"):
        kw = {}
        if op1 is not None:
            kw["op1"] = op1
        if accum is not None:
            kw["accum_out"] = _ap(accum)
        self.op(eng, lambda e: e.tensor_scalar(out=_ap(out), in0=_ap(in0), scalar1=_ap(s1), scalar2=_ap(s2), op0=op0, **kw),
                reads=_bufs(in0, s1, s2), writes=_bufs(out, accum))

    def stt(self, out, in0, scalar, in1, op0, op1):
        self.op("dve", lambda e: e.scalar_tensor_tensor(out=_ap(out), in0=_ap(in0), scalar=_ap(scalar), in1=_ap(in1), op0=op0, op1=op1),
                reads=_bufs(in0, scalar, in1), writes=_bufs(out))

    def cp(self, out, in_, eng="dve"):
        if eng == "act":
            self.op("act", lambda e: e.activation(out=_ap(out), in_=_ap(in_), func=AF.Copy), reads=_bufs(in_), writes=_bufs(out))
        else:
            self.op(eng, lambda e: e.tensor_copy(out=_ap(out), in_=_ap(in_)), reads=_bufs(in_), writes=_bufs(out))

    def red(self, out, in_, op):
        self.op("dve", lambda e: e.tensor_reduce(out=_ap(out), in_=_ap(in_), axis=AX, op=op), reads=_bufs(in_), writes=_bufs(out))

    def recip(self, out, in_):
        self.op("dve", lambda e: e.reciprocal(out=_ap(out), in_=_ap(in_)), reads=_bufs(in_), writes=_bufs(out))

    def memset(self, out, val, eng="dve"):
        self.op(eng, lambda e: e.memset(_ap(out), val), writes=_bufs(out))

    def dma(self, out, in_, eng="sp", slow=False):
        kw = {"allow_slow_non_contiguous": True} if slow else {}
        self.op(eng, lambda e: e.dma_start(out=_ap(out), in_=_ap(in_), **kw), reads=_bufs(in_), writes=_bufs(out), dma=True)

    def emit(self, final_waits=()):
        nc = self.nc
        with contextlib.ExitStack() as st:
            esem = {e: st.enter_context(nc.semaphore("s_" + e)) for e in self.ENG}
            dsem = [st.enter_context(nc.semaphore("d_%d" % i)) for i in range(len(self.slot_count))]
            for e in self.ENG:
                c = 0
                for o in self.per_eng[e]:
                    if o.signal and not o.is_dma:
                        c += 1
                        o.count = c
            block = st.enter_context(nc.Block())
            engh = {"pe": block.tensor, "act": block.scalar, "dve": block.vector,
                    "pool": block.gpsimd, "sp": block.sync}

            def mk(e):
                def body(eng):
                    known = {}
                    for o in self.per_eng[e]:
                        need = {}
                        for w in o.waits:
                            if w[0] == "dma":
                                s = dsem[w[1]]; v = 16 * w[2]
                            else:
                                s = esem[w[1].eng]; v = w[1].count
                            if v <= 0:
                                continue
                            k = id(s)
                            if k not in need or need[k][1] < v:
                                need[k] = (s, v)
                        for k, (s, v) in need.items():
                            if known.get(k, 0) >= v:
                                continue
                            eng.wait_ge(s, v)
                            known[k] = v
                        ins = o.fn(eng)
                        if PHASEMAP:
                            try:
                                self.phase_map[ins.ins.name] = o.phase
                            except Exception:
                                pass
                        if o.is_dma:
                            ins.then_inc(dsem[o.slot], 16)
                        elif o.signal:
                            ins.then_inc(esem[e], 1)
                    if e == "sp":
                        for i, cnt in enumerate(self.slot_count):
                            if cnt > 0:
                                eng.wait_ge(dsem[i], 16 * cnt)
                return body

            for e in self.ENG:
                engh[e](mk(e))
        if PHASEMAP:
            import json
            json.dump(self.phase_map, open(PHASEMAP, "w"))
        print("sched: ops", len(self.ops), "dma slots", len(self.slot_count), {e: len(v) for e, v in self.per_eng.items()}, flush=True)


class Ring:
    def __init__(self, bufs):
        self.bufs = bufs
        self.i = 0

    def next(self):
        b = self.bufs[self.i % len(self.bufs)]
        self.i += 1
        return b


def build_nc(nseq=2, stage="full", nlayers=DEPTH):
    nc = bass.Bass("TRN2", target_bir_lowering=False)
    S = Sched(nc)
    _uid = [0]

    def dram_in(name, shape):
        return Buf(nc.dram_tensor(name, list(shape), F32, kind="ExternalInput").ap(), name)

    def dram_scr(name, shape, dt=F32):
        b = Buf(nc.dram_tensor(name, list(shape), dt, kind="Internal").ap(), name)
        b.nowaw = True
        return b

    x_in = dram_in("x", [nseq, NLAT, D])
    ctx_in = dram_in("ctx", [nseq, NCTX, D])
    cT_in = dram_in("cT", [T, KT, 3])
    ada_w = dram_in("ada_w", [DEPTH, D, 6 * D])
    ada_b = dram_in("ada_b", [DEPTH, 6 * D])
    norm1_w = dram_in("norm1_w", [DEPTH, D])
    norm2_w = dram_in("norm2_w", [DEPTH, D])
    w_in = dram_in("w_in", [DEPTH, D, NIN])
    convw_h = dram_in("convw_h", [DEPTH, T, 16, 3])
    convb_h = dram_in("convb_h", [DEPTH, T, 16])
    dt_bias = dram_in("dt_bias", [DEPTH, 32])
    a_log = dram_in("a_log", [DEPTH, 32])
    d_skip = dram_in("d_skip", [DEPTH, 16])
    ssd_norm_w = dram_in("ssd_norm_w", [DEPTH, D])
    sgu_ln_w = dram_in("sgu_ln_w", [DEPTH, D])
    sgu_ln_b = dram_in("sgu_ln_b", [DEPTH, D])
    w_s = dram_in("w_s", [DEPTH, 8, T, T])
    b_s = dram_in("b_s", [DEPTH, 8 * T])
    w_pa = dram_in("w_pa", [DEPTH, D, D])
    w_pb = dram_in("w_pb", [DEPTH, D, D])
    w_o = dram_in("w_o", [DEPTH, D, D])
    router_w = dram_in("router_w", [DEPTH, D, NE])
    w_gate = dram_in("w_gate", [DEPTH, NE, D, D])
    w_up = dram_in("w_up", [DEPTH, NE, D, D])
    w_down = dram_in("w_down", [DEPTH, NE, D, D])
    final_norm_w = dram_in("final_norm_w", [1, D])
    y_out = Buf(nc.dram_tensor("y", [nseq, NLAT, D], F32, kind="ExternalOutput").ap(), "y")
    y_out.nowaw = True

    mod_d = dram_scr("mod_d", [DEPTH, 3, 6 * D])
    hA = [dram_scr(f"hA{s}", [NTOK, D]) for s in range(nseq)]
    hB = [dram_scr(f"hB{s}", [NTOK, D]) for s in range(nseq)]
    ybF = [dram_scr(f"ybF{s}", [NTOK, D]) for s in range(nseq)]
    ybT = [dram_scr(f"ybT{s}", [NTOK, D]) for s in range(nseq)]
    atb = [dram_scr(f"atb{s}", [NTOK // T, T, KT * T]) for s in range(nseq)]
    ywb = [dram_scr(f"ywb{s}", [NE, 2, T, D], BF16) for s in range(nseq)]
    ywc = [dram_scr(f"ywc{s}", [NE, 1, 32, D], BF16) for s in range(nseq)]
    dbg = {}

    def sb(st, name, shape, dt=F32):
        _uid[0] += 1
        nm = f"{name}_{_uid[0]}"
        return Buf(st.enter_context(nc.sbuf_tensor(nm, list(shape), dt)), nm)

    def ps(st, name, shape, dt=F32):
        _uid[0] += 1
        nm = f"{name}_{_uid[0]}"
        return Buf(st.enter_context(nc.psum_tensor(nm, list(shape), dt)), nm)

    def ring(st, name, shape, dt, n, psum=False):
        return Ring([(ps if psum else sb)(st, f"{name}{i}", shape, dt) for i in range(n)])

    with contextlib.ExitStack() as top:
        identF = sb(top, "identF", [T, T]); identB = sb(top, "identB", [T, T], BF16)
        Um = sb(top, "Um", [T, T]); Vm = sb(top, "Vm", [T, T]); Lf = sb(top, "Lf", [T, T]); Lb = sb(top, "Lb", [T, T])
        onesF = sb(top, "onesF", [T, T]); mhalf = sb(top, "mhalf", [T, 8])
        iota_slot = sb(top, "iota_slot", [T, 256]); iota_part = sb(top, "iota_part", [T, 2])

        def aff_sel(buf_v, pattern, cmp, fill, cm, base=0):
            S.op("pool", lambda e: e.affine_select(out=_ap(buf_v), in_=_ap(buf_v), pattern=pattern, compare_op=cmp,
                                                   fill=fill, base=base, channel_multiplier=cm),
                 reads=_bufs(buf_v), writes=_bufs(buf_v))

        S.memset(identF[:], 0.0, "pool"); aff_sel(identF[:], [[-1, T]], ALU.not_equal, 1.0, 1)
        S.cp(identB[:], identF[:], "pool")
        S.memset(onesF[:], 1.0, "pool"); S.memset(mhalf[:], -0.5, "pool")
        S.memset(Um[:], 1.0, "pool"); aff_sel(Um[:], [[1, T]], ALU.is_ge, 0.0, -1)
        S.memset(Vm[:], 1.0, "pool"); aff_sel(Vm[:], [[-1, T]], ALU.is_ge, 0.0, 1)
        S.memset(Lf[:], 1.0, "pool"); aff_sel(Lf[:], [[-1, T]], ALU.is_gt, 0.0, 1)
        S.memset(Lb[:], 1.0, "pool"); aff_sel(Lb[:], [[1, T]], ALU.is_gt, 0.0, -1)
        S.op("pool", lambda e: e.iota(_ap(iota_slot[:]), pattern=[[1, 256]], base=0, channel_multiplier=0,
                                      allow_small_or_imprecise_dtypes=True), writes=[iota_slot])
        S.op("pool", lambda e: e.iota(_ap(iota_part[:]), pattern=[[T, 2]], base=0, channel_multiplier=1,
                                      allow_small_or_imprecise_dtypes=True), writes=[iota_part])

        def mod_gen(st, l, depth=2):
            cT = sb(st, "cT", [T, KT, 3]); sg = sb(st, "sg", [T, KT, 3])
            scT = sb(st, "scT", [T, KT, T])
            awr = ring(st, "aw", [T, KT, 512], F32, depth)
            abr = ring(st, "ab", [3, 512], F32, 2)
            mor = ring(st, "mo", [3, 512], F32, 2)
            pm = ring(st, "pm", [T, 512], F32, 2, psum=True)
            S.dma(cT[:], cT_in[:])
            S.memset(scT[:], 0.0)
            S.act(sg[:], cT[:], AF.Sigmoid)
            S.tt(scT[:, :, 0:3], cT[:], sg[:], ALU.mult)
            for nb in range(12):
                aw = awr.next(); ab = abr.next(); mo = mor.next(); p = pm.next()
                S.dma(aw[:], ada_w[l].rr("(k p) n -> p k n", p=T)[:, :, nb * 512:(nb + 1) * 512])
                S.dma(ab[:], ada_b[l:l + 1, nb * 512:(nb + 1) * 512].bc([3, 512]))
                for k in range(KT):
                    S.mm(p[:], scT[:, k, :], aw[:, k, :], start=(k == 0), stop=(k == KT - 1))
                S.tt(mo[:], p[0:3, :], ab[:], ALU.add)
                S.dma(mod_d[l, :, nb * 512:(nb + 1) * 512], mo[:], eng="pool")
                yield

        with contextlib.ExitStack() as st:
            for _ in mod_gen(st, 0, depth=4):
                pass
        S.barrier()

        def rms_rstd(st_small, ssum, n, rstd_out):
            S.ts(rstd_out, ssum, 1.0 / n, EPS, op0=ALU.mult, op1=ALU.add)
            S.tt(rstd_out, rstd_out, mhalf[:, 0:1], ALU.pow, eng="pool")

        def load_vec_fm(dst, l, v, j):
            S.dma(dst, mod_d[l, v, j * D:(j + 1) * D].rr("(k p) -> p k", p=T), slow=True)

        def load_vec_bc(dst, l, v, j):
            S.dma(dst, mod_d[l, v:v + 1, j * D:(j + 1) * D].bc([T, D]))

        def mixer(l, s, h_src_lat, h_src_ctx, h_dst, last):
            with contextlib.ExitStack() as mx:
                xmT_l = sb(mx, "xmTl", [T, KT, NLAT + 2], BF16)
                xmT_c = sb(mx, "xmTc", [T, KT, NCTX + 2], BF16)
                s1 = sb(mx, "s1", [T, 2, KT]); sh1 = sb(mx, "sh1", [T, 2, KT])
                n1w = sb(mx, "n1w", [T, KT])
                dtraw = sb(mx, "dtraw", [T, 18, 32])
                S.dma(n1w[:], norm1_w[l].rr("(k p) -> p k", p=T), slow=True)
                for i, v in enumerate((s, 2)):
                    load_vec_fm(sh1[:, i, :], l, v, 0)
                    load_vec_fm(s1[:, i, :], l, v, 1)
                S.ts(s1[:], s1[:], 1.0, None, op0=ALU.add)
                S.tt(s1[:], s1[:], n1w[:].us(1).bc([T, 2, KT]), ALU.mult)
                S.memset(xmT_l[:, :, 0:1], 0.0); S.memset(xmT_l[:, :, NLAT + 1:NLAT + 2], 0.0)
                S.memset(xmT_c[:, :, 0:1], 0.0); S.memset(xmT_c[:, :, NCTX + 1:NCTX + 2], 0.0)

                S.phase = f"L{l}s{s}.M1"
                with contextlib.ExitStack() as st:
                    hin = ring(st, "hin", [T, D], F32, 2); xn = ring(st, "xn", [T, D], F32, 2)
                    sq = sb(st, "sq", [T, D]); stat = ring(st, "stat", [T, 2], F32, 2)
                    ptr = ring(st, "ptr", [T, D], F32, 2, psum=True)
                    items = [(h_src_ctx, c, xmT_c, 1) for c in range(2)] + [(h_src_lat, c, xmT_l, 0) for c in range(16)]

                    def m1_s1(it):
                        src, c, xmT, vi = it
                        h_ = hin.next(); x_ = xn.next(); sa = stat.next(); p = ptr.next()
                        S.dma(h_[:], src[c * T:(c + 1) * T, :])
                        S.act(sq[:], h_[:], AF.Square, accum=sa[:, 0:1])
                        rms_rstd(None, sa[:, 0:1], D, sa[:, 1:2])
                        S.ts(x_[:], h_[:], sa[:, 1:2], None, op0=ALU.mult)
                        for k in range(KT):
                            S.tr(p[:, k * T:(k + 1) * T], x_[:, k * T:(k + 1) * T], identF[:])
                        return p

                    def m1_s2(it, p):
                        src, c, xmT, vi = it
                        for k in range(KT):
                            S.act(xmT[:, k, 1 + c * T:1 + (c + 1) * T], p[:, k * T:(k + 1) * T], AF.Identity,
                                  bias=sh1[:, vi, k:k + 1], scale=s1[:, vi, k:k + 1])

                    prev = None
                    for it in items:
                        p_ = m1_s1(it)
                        if prev is not None:
                            m1_s2(*prev)
                        prev = (it, p_)
                    m1_s2(*prev)
                S.barrier()
                if stage == "m1":
                    return

                with contextlib.ExitStack() as sd:
                    xBC_l = sb(sd, "xBCl", [T, 16, NLAT], BF16)
                    xBC_c = sb(sd, "xBCc", [T, 16, NCTX], BF16)
                    Sf = sb(sd, "Sf", [T, D]); Sb_ = sb(sd, "Sb", [T, D])
                    S.phase = f"L{l}s{s}.M2"
                    with contextlib.ExitStack() as st:
                        wx = sb(st, "wx", [T, KT, 2080], BF16)
                        cw = sb(st, "cw", [T, 16, 3]); cb = sb(st, "cb", [T, 16])
                        ctmp = ring(st, "ctmp", [T, 512], F32, 2)
                        pdt = ring(st, "pdt", [T, 32], F32, 2, psum=True)
                        S.dma(wx[:], w_in[l].rr("(k p) n -> p k n", p=T)[:, :, 0:2080], eng="pool")
                        S.dma(cw[:], convw_h[l]); S.dma(cb[:], convb_h[l])
                        mg = mod_gen(st, l + 1) if (s == 0 and l + 1 < nlayers) else None
                        pp = ring(st, "pp", [T, 512], F32, 2 if mg is not None else 4, psum=True)
                        for (nt, xmT, xBC, c0) in ((NCTX, xmT_c, xBC_c, 0), (NLAT, xmT_l, xBC_l, 2)):
                            for c in range(nt // T):
                                p = pdt.next()
                                for k in range(KT):
                                    S.mm(p[:], xmT[:, k, 1 + c * T:1 + (c + 1) * T], wx[:, k, 2048:2080],
                                         start=(k == 0), stop=(k == KT - 1))
                                S.cp(dtraw[:, c0 + c, :], p[:], "act")
                            t0 = 0
                            while t0 < nt:
                                nw = min(510, nt - t0)
                                for q in range(16):
                                    p = pp.next(); tm = ctmp.next()
                                    for k in range(KT):
                                        S.mm(p[:, 0:nw + 2], wx[:, k, q * T:(q + 1) * T], xmT[:, k, t0:t0 + nw + 2],
                                             start=(k == 0), stop=(k == KT - 1))
                                    S.act(tm[:, 0:nw], p[:, 1:nw + 1], AF.Identity, bias=cb[:, q:q + 1], scale=cw[:, q, 1:2])
                                    S.stt(tm[:, 0:nw], p[:, 0:nw], cw[:, q, 0:1], tm[:, 0:nw], ALU.mult, ALU.add)
                                    S.stt(tm[:, 0:nw], p[:, 2:nw + 2], cw[:, q, 2:3], tm[:, 0:nw], ALU.mult, ALU.add)
                                    S.act(xBC[:, q, t0:t0 + nw], tm[:, 0:nw], AF.Silu)
                                    if mg is not None and q % 4 == 3:
                                        try:
                                            next(mg)
                                        except StopIteration:
                                            mg = None
                                t0 += nw
                        if mg is not None:
                            for _ in mg:
                                pass
                    S.barrier()
                    if stage == "m2":
                        dbg["xBC"] = (xBC_l, mx, sd)
                        return

                    S.phase = f"L{l}s{s}.M3"
                    with contextlib.ExitStack() as st:
                        dtb = sb(st, "dtb", [T, 32]); Abc = sb(st, "Abc", [T, 32]); dsk = sb(st, "dsk", [T, 16])
                        xtok = ring(st, "xtok", [T, D], BF16, 2); btok = ring(st, "btok", [T, 512], BF16, 2)
                        dtr = ring(st, "dt", [T, 32], F32, 2); dtAr = ring(st, "dtA", [T, 32], F32, 2)
                        sm = ring(st, "sm", [T, 8, 16], F32, 2)
                        DmR = ring(st, "Dm", [T, 1024], F32, 2); EmR = ring(st, "Em", [T, 1024], F32, 2)
                        MT = ring(st, "MT", [T, 1024], BF16, 4)
                        cbm = ring(st, "cbm", [T, 512], F32, 2)
                        xdt = ring(st, "xdt", [T, D], BF16, 2); xw = ring(st, "xw", [T, D], BF16, 2)
                        Sbf = ring(st, "Sbf", [T, D], BF16, 2)
                        ytmp = ring(st, "ytmp", [T, D], F32, 2); yprev = ring(st, "yprev", [T, D], F32, 2)
                        PA = ps(st, "PA", [T, 1024]); PB = ps(st, "PB", [T, 1024]); PC = ps(st, "PC", [T, 1024])
                        PD = ps(st, "PD", [T, 64]); PT = ps(st, "PT", [T, 1024], BF16)
                        S.dma(dtb[:], dt_bias[l:l + 1, :].bc([T, 32]))
                        S.dma(Abc[:], a_log[l:l + 1, :].bc([T, 32]))
                        S.dma(dsk[:], d_skip[l:l + 1, :].bc([T, 16]))
                        S.act(Abc[:], Abc[:], AF.Exp)
                        S.ts(Abc[:], Abc[:], -1.0, None, op0=ALU.mult)
                        S.memset(Sf[:], 0.0); S.memset(Sb_[:], 0.0)

                        def scan_step(nch, xBC, c0, tok0, direction, need_y, c):
                            fwd = direction == 0
                            St = Sf if fwd else Sb_
                            Lm = Lf if fwd else Lb
                            Cm = Um if fwd else Vm
                            mask = Um if fwd else Vm
                            hs = slice(0, 16) if fwd else slice(16, 32)
                            tsl = slice(c * T, (c + 1) * T)
                            xt = xtok.next(); bt = btok.next(); dt_ = dtr.next(); dA = dtAr.next(); sm_ = sm.next()
                            for k in range(8):
                                S.tr(PT[:, k * T:(k + 1) * T], xBC[:, k, tsl], identB[:])
                            S.cp(xt[:], PT[:], "act")
                            S.tt(dt_[:], dtraw[:, c0 + c, :], dtb[:], ALU.add)
                            S.act(dA[:], dt_[:], AF.Abs)
                            S.act(dA[:], dA[:], AF.Exp, scale=-1.0)
                            S.act(dA[:], dA[:], AF.Ln, bias=1.0)
                            S.ts(dt_[:], dt_[:], 0.0, None, op0=ALU.max)
                            S.tt(dt_[:], dt_[:], dA[:], ALU.add)
                            S.tt(dA[:], dt_[:], Abc[:], ALU.mult)
                            yield
                            for g in range(4):
                                S.tr(PT[:, g * T:(g + 1) * T], xBC[:, 8 + g, tsl], identB[:])
                            S.cp(bt[:], PT[:, 0:512], "act")
                            S.mm(PD[:, 0:16], Cm[:], dA[:, hs])
                            S.mm(PD[:, 16:32], onesF[:], dA[:, hs])
                            S.cp(sm_[:, 0:2, :], PD[:, 0:32].rr("p (a b) -> p a b", a=2))
                            S.act(sm_[:, 2, :], sm_[:, 0, :], AF.Exp)
                            S.tt(sm_[:, 3, :], sm_[:, 1, :], sm_[:, 0, :], ALU.subtract)
                            S.act(sm_[:, 3, :], sm_[:, 3, :], AF.Exp)
                            S.act(sm_[:, 4, :], sm_[:, 1, :], AF.Exp)
                            S.tt(sm_[:, 5, :], sm_[:, 3, :], dt_[:, hs], ALU.mult)
                            xd = xdt.next(); xw_ = xw.next()
                            S.tt(xd[:].rr("p (h q) -> p h q", h=16), xt[:].rr("p (h q) -> p h q", h=16),
                                 dt_[:, hs].us(2).bc([T, 16, 64]), ALU.mult, eng=POOL_ENG)
                            S.tt(xw_[:].rr("p (h q) -> p h q", h=16), xt[:].rr("p (h q) -> p h q", h=16),
                                 sm_[:, 5, :].us(2).bc([T, 16, 64]), ALU.mult, eng=POOL_ENG)
                            yield
                            if need_y:
                                for g in range(4):
                                    S.mm(PC[:, g * T:(g + 1) * T], xBC[:, 8 + g, tsl], xBC[:, 12 + g, tsl])
                                cm_ = cbm.next()
                                S.tt(cm_[:].rr("p (g i) -> p g i", g=4), PC[:, 0:512].rr("p (g i) -> p g i", g=4),
                                     mask[:].us(1).bc([T, 4, T]), ALU.mult)
                                mts = []
                                for half in range(2):
                                    Dm = DmR.next(); Em = EmR.next()
                                    hsel = slice(hs.start + half * 8, hs.start + half * 8 + 8)
                                    if half == 0:
                                        S.tt(Dm[:].rr("p (h i) -> p h i", h=8), Cm[:].us(1).bc([T, 8, T]),
                                             dA[:, hsel].us(2).bc([T, 8, T]), ALU.mult)
                                    else:
                                        for h8 in range(8):
                                            hcol = hs.start + half * 8 + h8
                                            S.act(Dm[:, h8 * T:(h8 + 1) * T], Cm[:], AF.Copy, scale=dA[:, hcol:hcol + 1])
                                    yield
                                    for b2 in range(2):
                                        S.mm(PA[:, b2 * 512:(b2 + 1) * 512], Lm[:], Dm[:, b2 * 512:(b2 + 1) * 512])
                                    S.act(Em[:], PA[:], AF.Exp)
                                    mt = MT.next()
                                    S.tt(mt[:].rr("p (g r i) -> p g r i", g=2, r=4),
                                         Em[:].rr("p (g r i) -> p g r i", g=2, r=4),
                                         cm_[:, half * 256:(half + 1) * 256].rr("p (g i) -> p g i", g=2).us(2).bc([T, 2, 4, T]),
                                         ALU.mult)
                                    mts.append(mt)
                                sbf = Sbf.next()
                                S.cp(sbf[:], St[:], "act")
                                yt = ytmp.next()
                                if fwd:
                                    yp = yprev.next()
                                    S.tt(yp[:].rr("p (h q) -> p h q", h=16), xt[:].rr("p (h q) -> p h q", h=16),
                                         dsk[:].us(2).bc([T, 16, 64]), ALU.mult, eng=POOL_ENG)
                                yield
                                for g in range(4):
                                    S.mm(PC[:, g * 256:(g + 1) * 256], xBC[:, 12 + g, tsl], sbf[:, g * 256:(g + 1) * 256])
                                S.tt(yt[:].rr("p (h q) -> p h q", h=16), PC[:].rr("p (h q) -> p h q", h=16),
                                     sm_[:, 2, :].us(2).bc([T, 16, 64]), ALU.mult)
                                if fwd:
                                    S.tt(yt[:], yt[:], yp[:], ALU.add)
                                yield
                                for h in range(16):
                                    mt = mts[h // 8]; hh_ = h % 8
                                    S.mm(PB[:, h * 64:(h + 1) * 64], mt[:, hh_ * T:(hh_ + 1) * T], xd[:, h * 64:(h + 1) * 64])
                                S.tt(yt[:], yt[:], PB[:], ALU.add)
                                rows = slice(tok0 + c * T, tok0 + (c + 1) * T)
                                S.dma((ybF if fwd else ybT)[s][rows, :], yt[:])
                                yield
                            S.tt(St[:].rr("p (h q) -> p h q", h=16), St[:].rr("p (h q) -> p h q", h=16),
                                 sm_[:, 4, :].us(2).bc([T, 16, 64]), ALU.mult)
                            for g in range(4):
                                S.mm(PC[:, g * 256:(g + 1) * 256], bt[:, g * T:(g + 1) * T], xw_[:, g * 256:(g + 1) * 256])
                            S.tt(St[:], St[:], PC[:], ALU.add)

                        def lockstep(ga, gb):
                            live = [ga, gb]
                            while live:
                                for g_ in list(live):
                                    try:
                                        next(g_)
                                    except StopIteration:
                                        live.remove(g_)

                        for i_ in range(2):
                            lockstep(scan_step(2, xBC_c, 0, 0, 0, not last, i_), scan_step(2, xBC_c, 0, 0, 1, not last, 1 - i_))
                        for i_ in range(16):
                            lockstep(scan_step(16, xBC_l, 2, NCTX, 0, True, i_), scan_step(16, xBC_l, 2, NCTX, 1, True, 15 - i_))
                    S.barrier()
                if stage == "m3":
                    return

                tok_sets = ([(0, 2, xmT_c, 1)] if not last else []) + [(NCTX, 16, xmT_l, 0)]
                items = [(tok0, c, xmT, vi) for (tok0, nch, xmT, vi) in tok_sets for c in range(nch)]

                def pipeline(s1f, s2f):
                    prev = None
                    for it in items + [None]:
                        pieces, cx = s1f(it) if it is not None else ([], None)
                        g = s2f(*prev) if prev is not None else None
                        pieces = list(pieces)
                        while g is not None or pieces:
                            if pieces:
                                pieces.pop(0)()
                            if g is not None:
                                try:
                                    next(g)
                                except StopIteration:
                                    g = None
                        prev = (it, cx)

                S.phase = f"L{l}s{s}.M4a"
                with contextlib.ExitStack() as st:
                    wz = sb(st, "wz", [T, KT, D], BF16); wga = sb(st, "wga", [T, KT, D], BF16); wpa = sb(st, "wpa", [T, KT, D], BF16)
                    snw = sb(st, "snw", [T, D])
                    yin = ring(st, "yin", [T, D], F32, 2); yin2 = ring(st, "yin2", [T, D], F32, 2)
                    zsR = ring(st, "zs", [T, D], F32, 2); gaR = ring(st, "gaT", [T, D], F32, 2)
                    gg = sb(st, "gg", [T, D]); sqs = sb(st, "sqs", [T, 256])
                    gst = ring(st, "gst", [T, 8], F32, 2)
                    stokR = ring(st, "stok", [T, D], BF16, 2); sT = sb(st, "sT", [T, KT, T], BF16)
                    AT = ring(st, "AT", [T, KT, T], F32, 2)
                    PZ = ps(st, "PZ", [T, D]); PT2 = ps(st, "PT2", [T, D], BF16); PPA = ps(st, "PPA", [T, D]); PGA = ps(st, "PGA", [T, D])
                    wv = w_in[l].rr("(k p) n -> p k n", p=T)
                    S.dma(wz[:], wv[:, :, 2080:3104], eng="pool")
                    S.dma(wga[:], wv[:, :, 5152:6176], eng="pool")
                    S.dma(wpa[:], w_pa[l].rr("(k p) n -> p k n", p=T), eng="pool")
                    S.dma(snw[:], ssd_norm_w[l:l + 1, :].bc([T, D]))

                    def a_s1(it):
                        tok0, c, xmT, vi = it
                        rows = slice(tok0 + c * T, tok0 + (c + 1) * T)
                        csl = slice(1 + c * T, 1 + (c + 1) * T)
                        y_ = yin.next(); y2_ = yin2.next(); zs = zsR.next(); gaT = gaR.next()
                        gs = gst.next(); stok = stokR.next()

                        def pz():
                            S.dma(y_[:], ybF[s][rows, :])
                            S.dma(y2_[:], ybT[s][rows, :])
                            for nh in range(2):
                                for k in range(KT):
                                    S.mm(PZ[:, nh * 512:(nh + 1) * 512], xmT[:, k, csl], wz[:, k, nh * 512:(nh + 1) * 512],
                                         start=(k == 0), stop=(k == KT - 1))
                            S.act(zs[:], PZ[:], AF.Silu)
                            S.tt(y_[:], y_[:], y2_[:], ALU.add)
                            S.tt(gg[:], y_[:], zs[:], ALU.mult)
                            for g in range(4):
                                S.act(sqs[:], gg[:, g * 256:(g + 1) * 256], AF.Square, accum=gs[:, g:g + 1])
                            S.ts(gs[:, 4:8], gs[:, 0:4], 1.0 / 256, EPS, op0=ALU.mult, op1=ALU.add)
                            S.tt(gs[:, 4:8], gs[:, 4:8], mhalf[:, 0:4], ALU.pow, eng="pool")
                            S.tt(gg[:].rr("p (g q) -> p g q", g=4), gg[:].rr("p (g q) -> p g q", g=4),
                                 gs[:, 4:8].us(2).bc([T, 4, 256]), ALU.mult)
                            S.tt(stok[:], gg[:], snw[:], ALU.mult)

                        def pga():
                            for dt_ in range(KT):
                                for k in range(KT):
                                    S.mm(PGA[:, dt_ * T:(dt_ + 1) * T], wga[:, k, dt_ * T:(dt_ + 1) * T], xmT[:, k, csl],
                                         start=(k == 0), stop=(k == KT - 1))
                            S.act(gaT[:], PGA[:], AF.Tanh, scale=0.5)
                        return [pz, pga], (stok, gaT)

                    def a_s2(it, cx):
                        tok0, c, xmT, vi = it
                        stok, gaT = cx
                        rows = slice(tok0 + c * T, tok0 + (c + 1) * T)
                        at = AT.next()
                        for k in range(KT):
                            S.tr(PT2[:, k * T:(k + 1) * T], stok[:, k * T:(k + 1) * T], identB[:])
                        S.cp(sT[:].rr("p k t -> p (k t)"), PT2[:], "act")
                        yield
                        for dt_ in range(KT):
                            for k in range(KT):
                                S.mm(PPA[:, dt_ * T:(dt_ + 1) * T], wpa[:, k, dt_ * T:(dt_ + 1) * T], sT[:, k, :],
                                     start=(k == 0), stop=(k == KT - 1))
                        S.stt(at[:].rr("p k t -> p (k t)"), gaT[:], 1.0, PPA[:], ALU.add, ALU.mult)
                        S.dma(atb[s][(tok0 // T) + c], at[:].rr("p k t -> p (k t)"), eng="pool")

                    pipeline(a_s1, a_s2)
                S.barrier()
                if stage == "m4a":
                    return

                S.phase = f"L{l}s{s}.M4b"
                with contextlib.ExitStack() as st:
                    wu = sb(st, "wu", [T, KT, D], BF16); wvv = sb(st, "wvv", [T, KT, D], BF16); wgb = sb(st, "wgb", [T, KT, D], BF16)
                    wpb = sb(st, "wpb", [T, KT, D], BF16); wo = sb(st, "wo", [T, KT, D], BF16)
                    lnw = sb(st, "lnw", [T, D]); lnb = sb(st, "lnb", [T, D]); bsb = sb(st, "bsb", [T, D]); g1b = sb(st, "g1b", [T, 2, D])
                    wsT = sb(st, "wsT", [T, 8, T], BF16)
                    Q0 = ps(st, "Q0", [T, D]); Q2 = ps(st, "Q2", [T, D]); QG = ps(st, "QG", [T, D]); QS = ps(st, "QS", [T, D])
                    wv = w_in[l].rr("(k p) n -> p k n", p=T)
                    S.dma(wvv[:], wv[:, :, 4128:5152], eng="pool")
                    S.dma(wu[:], wv[:, :, 3104:4128], eng="pool")
                    S.dma(wgb[:], wv[:, :, 6176:7200], eng="pool")
                    S.dma(wpb[:], w_pb[l].rr("(k p) n -> p k n", p=T), eng="pool")
                    S.dma(wo[:], w_o[l].rr("(k p) n -> p k n", p=T), eng="pool")
                    S.dma(lnw[:], sgu_ln_w[l:l + 1, :].bc([T, D]))
                    S.dma(lnb[:], sgu_ln_b[l:l + 1, :].bc([T, D]))
                    S.dma(bsb[:], b_s[l:l + 1, :].bc([T, D]))
                    for i, v in enumerate((s, 2)):
                        load_vec_bc(g1b[:, i, :], l, v, 2)
                    S.ts(g1b[:], g1b[:], 0.5, None, op0=ALU.mult)
                    with contextlib.ExitStack() as st2:
                        wsl = sb(st2, "wsl", [T, 8, T])
                        S.dma(wsl[:], w_s[l].rr("g i j -> i g j"))
                        for g in range(8):
                            S.tr(Q0[:, g * T:(g + 1) * T], wsl[:, g, :], identF[:])
                        S.cp(wsT[:].rr("p g i -> p (g i)"), Q0[:])
                    S.barrier()
                    vvR = ring(st, "vv", [T, D], F32, 1); uTR = ring(st, "uT", [T, D], F32, 2); gbR = ring(st, "gbT", [T, D], F32, 2)
                    vnR = ring(st, "vn", [T, D], BF16, 2); tmpv = sb(st, "tmpv", [T, D])
                    sgT = sb(st, "sgT", [T, KT, T], BF16); mT = sb(st, "mT", [T, KT, T], BF16)
                    bst = ring(st, "bst", [T, 16], F32, 2)
                    ATi = ring(st, "ATi", [T, KT, T], F32, 2)
                    hin = ring(st, "hin2", [T, D], F32, 2)

                    def b_s1(it):
                        tok0, c, xmT, vi = it
                        rows = slice(tok0 + c * T, tok0 + (c + 1) * T)
                        csl = slice(1 + c * T, 1 + (c + 1) * T)
                        ati = ATi.next(); h_ = hin.next(); vv = vvR.next(); uT = uTR.next(); gbT = gbR.next()
                        vn = vnR.next(); bs_ = bst.next()
                        src = (h_src_ctx if tok0 == 0 else h_src_lat)

                        def pv():
                            S.dma(ati[:].rr("p k t -> p (k t)"), atb[s][(tok0 // T) + c])
                            S.dma(h_[:], src[c * T:(c + 1) * T, :])
                            for nh in range(2):
                                for k in range(KT):
                                    S.mm(Q0[:, nh * 512:(nh + 1) * 512], xmT[:, k, csl], wvv[:, k, nh * 512:(nh + 1) * 512],
                                         start=(k == 0), stop=(k == KT - 1))
                            S.act(vv[:], Q0[:], AF.Gelu)

                        def pln():
                            for i2 in range(2):
                                S.op("dve", lambda e, i2=i2: e.bn_stats(out=_ap(bs_[:, i2 * 6:(i2 + 1) * 6]),
                                                                        in_=_ap(vv[:, i2 * 512:(i2 + 1) * 512])),
                                     reads=[vv], writes=[bs_])
                            S.op("dve", lambda e: e.bn_aggr(out=_ap(bs_[:, 12:14]), in_=_ap(bs_[:, 0:12])),
                                 reads=[bs_], writes=[bs_])
                            S.ts(bs_[:, 14:15], bs_[:, 13:14], 1.0, EPS, op0=ALU.mult, op1=ALU.add)
                            S.tt(bs_[:, 14:15], bs_[:, 14:15], mhalf[:, 0:1], ALU.pow, eng="pool")
                            S.ts(vv[:], vv[:], bs_[:, 12:13], bs_[:, 14:15], op0=ALU.subtract, op1=ALU.mult)
                            S.tt(vv[:], vv[:], lnw[:], ALU.mult)
                            S.tt(vn[:], vv[:], lnb[:], ALU.add)

                        def pu():
                            for dt_ in range(KT):
                                for k in range(KT):
                                    S.mm(Q2[:, dt_ * T:(dt_ + 1) * T], wu[:, k, dt_ * T:(dt_ + 1) * T], xmT[:, k, csl],
                                         start=(k == 0), stop=(k == KT - 1))
                            S.act(uT[:], Q2[:], AF.Gelu)

                        def pgb():
                            for dt_ in range(KT):
                                for k in range(KT):
                                    S.mm(QG[:, dt_ * T:(dt_ + 1) * T], wgb[:, k, dt_ * T:(dt_ + 1) * T], xmT[:, k, csl],
                                         start=(k == 0), stop=(k == KT - 1))
                            S.act(gbT[:], QG[:], AF.Tanh, scale=0.5)
                        return [pv, pu, pgb, pln], (ati, h_, vn, uT, gbT)

                    def b_s2(it, cx):
                        tok0, c, xmT, vi = it
                        ati, h_, vn, uT, gbT = cx
                        vv = tmpv
                        rows = slice(tok0 + c * T, tok0 + (c + 1) * T)
                        for g in range(8):
                            S.mm(QS[:, g * T:(g + 1) * T], vn[:, g * T:(g + 1) * T], wsT[:, g, :])
                        yield
                        S.tt(vv[:], QS[:], bsb[:], ALU.add)
                        S.tt(sgT[:].rr("p k t -> p (k t)"), uT[:], vv[:], ALU.mult)
                        for dt_ in range(KT):
                            for k in range(KT):
                                S.mm(QS[:, dt_ * T:(dt_ + 1) * T], wpb[:, k, dt_ * T:(dt_ + 1) * T], sgT[:, k, :],
                                     start=(k == 0), stop=(k == KT - 1))
                        yield
                        S.stt(gbT[:], gbT[:], 1.0, QS[:], ALU.add, ALU.mult)
                        S.tt(mT[:].rr("p k t -> p (k t)"), gbT[:], ati[:].rr("p k t -> p (k t)"), ALU.add)
                        for nh in range(2):
                            for k in range(KT):
                                S.mm(QS[:, nh * 512:(nh + 1) * 512], mT[:, k, :], wo[:, k, nh * 512:(nh + 1) * 512],
                                     start=(k == 0), stop=(k == KT - 1))
                        yield
                        S.tt(vv[:], QS[:], g1b[:, vi, :], ALU.mult)
                        S.tt(h_[:], h_[:], vv[:], ALU.add)
                        S.dma(h_dst[rows, :], h_[:], eng="pool")

                    pipeline(b_s1, b_s2)
                S.barrier()

        def moe(l, s, sets, h_src):
            with contextlib.ExitStack() as mo:
                for ts_ in sets:
                    nch = ts_["nch"]; n = nch * T
                    ts_["CT"] = (ts_["cap"] + T - 1) // T
                    ts_["CP"] = min(ts_["cap"], T)
                    ts_["xf"] = sb(mo, "xf", [T, nch, D], BF16)
                    ts_["affhl"] = sb(mo, "affhl", [T, nch, NE, 2], BF16)
                    ts_["posm_tok"] = sb(mo, "posmtok", [T, nch, NE])
                    ts_["posmT"] = sb(mo, "posmT", [T, n], BF16)
                wst = contextlib.ExitStack()
                wr = ring(wst, "wexp", [T, KT, D], BF16, 4)
                pre_w = []
                for wsrc in (w_gate, w_up, w_down):
                    w_ = wr.next(); S.dma(w_[:], wsrc[l, 0].rr("(k p) f -> p k f", p=T), eng="pool"); pre_w.append(w_)
                S.phase = f"L{l}s{s}.moeR"
                for ts_ in sets:
                    nch = ts_["nch"]; n = nch * T; v = ts_["v"]; tok0 = ts_["tok0"]; cap = ts_["cap"]
                    xf = ts_["xf"]; affhl = ts_["affhl"]; posm_tok = ts_["posm_tok"]; posmT = ts_["posmT"]
                    with contextlib.ExitStack() as st:
                        s2b = sb(st, "s2b", [T, D]); sh2b = sb(st, "sh2b", [T, D]); n2w = sb(st, "n2w", [T, D])
                        rw = sb(st, "rw", [T, KT, NE])
                        hin = ring(st, "hin3", [T, D], F32, 2); xff = ring(st, "xff", [T, D], F32, 2)
                        sq = sb(st, "sq3", [T, D]); stat = ring(st, "stat3", [T, 2], F32, 2)
                        xfT = ring(st, "xfT", [T, KT, T], F32, 2)
                        aff = sb(st, "aff", [T, nch, NE]); lg = sb(st, "lg", [T, nch, NE]); sm = sb(st, "smx", [T, 2, nch])
                        affT = sb(st, "affT", [NE, n]); maskT = sb(st, "maskT", [NE, n]); inclT = sb(st, "inclT", [NE, n])
                        onesT = sb(st, "onesT", [NE, n]); pmf = sb(st, "pmf", [NE, n])
                        bis = sb(st, "bis", [NE, 4])
                        ptr = ring(st, "ptr3", [T, D], F32, 2, psum=True)
                        plg = ps(st, "plg", [T, nch, NE]); pat = ps(st, "pat", [NE, 512]); ppt = ps(st, "ppt", [T, 512])
                        load_vec_bc(sh2b[:], l, v, 3)
                        load_vec_bc(s2b[:], l, v, 4)
                        S.dma(n2w[:], norm2_w[l:l + 1, :].bc([T, D]))
                        S.dma(rw[:], router_w[l].rr("(k p) e -> p k e", p=T))
                        S.ts(s2b[:], s2b[:], 1.0, None, op0=ALU.add)
                        S.tt(s2b[:], s2b[:], n2w[:], ALU.mult)

                        def r_s1(c):
                            h_ = hin.next(); x_ = xff.next(); sa = stat.next(); p = ptr.next()
                            S.dma(h_[:], h_src[tok0 + c * T:tok0 + (c + 1) * T, :])
                            S.act(sq[:], h_[:], AF.Square, accum=sa[:, 0:1])
                            rms_rstd(None, sa[:, 0:1], D, sa[:, 1:2])
                            S.stt(x_[:], h_[:], sa[:, 1:2], s2b[:], ALU.mult, ALU.mult)
                            S.tt(x_[:], x_[:], sh2b[:], ALU.add)
                            S.cp(xf[:, c, :], x_[:], "act")
                            for k in range(KT):
                                S.tr(p[:, k * T:(k + 1) * T], x_[:, k * T:(k + 1) * T], identF[:])
                            return p

                        def r_s2(c, p):
                            xt_ = xfT.next()
                            S.cp(xt_[:].rr("p k t -> p (k t)"), p[:], "act")
                            for k in range(KT):
                                S.mm(plg[:, c, :], xt_[:, k, :], rw[:, k, :], start=(k == 0), stop=(k == KT - 1))

                        prev = None
                        for c in range(nch):
                            p_ = r_s1(c)
                            if prev is not None:
                                r_s2(*prev)
                            prev = (c, p_)
                        r_s2(*prev)
                        S.cp(lg[:], plg[:])
                        S.red(sm[:, 0, :], lg[:], ALU.max)
                        S.tt(lg[:], lg[:], sm[:, 0, :].us(2).bc([T, nch, NE]), ALU.subtract)
                        S.act(lg[:], lg[:], AF.Exp)
                        S.red(sm[:, 1, :], lg[:], ALU.add)
                        S.recip(sm[:, 1, :], sm[:, 1, :])
                        S.tt(aff[:], lg[:], sm[:, 1, :].us(2).bc([T, nch, NE]), ALU.mult)
                        S.cp(affhl[:, :, :, 0], aff[:])
                        S.tt(lg[:], aff[:], affhl[:, :, :, 0], ALU.subtract)
                        S.cp(affhl[:, :, :, 1], lg[:])
                        for c in range(nch):
                            off = (c % 4) * T
                            S.tr(pat[:, off:off + T], aff[:, c, :], identF[:])
                            if c % 4 == 3 or c == nch - 1:
                                w_ = off + T
                                S.cp(affT[:, (c // 4) * 512:(c // 4) * 512 + w_], pat[:, 0:w_])
                        S.memset(bis[:], 0.0)
                        S.memset(onesT[:], 1.0)
                        for it in range(NBIS):
                            half = 2.0 ** (-(it + 1))
                            S.ts(bis[:, 1:2], bis[:, 0:1], half, None, op0=ALU.add)
                            S.ts(maskT[:], affT[:], bis[:, 1:2], None, op0=ALU.is_ge, op1=ALU.add, accum=bis[:, 2:3])
                            S.ts(bis[:, 3:4], bis[:, 2:3], float(cap), half, op0=ALU.is_ge, op1=ALU.mult)
                            S.tt(bis[:, 0:1], bis[:, 0:1], bis[:, 3:4], ALU.add)
                        S.ts(maskT[:], affT[:], bis[:, 0:1], None, op0=ALU.is_ge)
                        S.op("dve", lambda e, inclT=inclT, onesT=onesT, maskT=maskT: e.tensor_tensor_scan(
                            out=_ap(inclT[:]), data0=_ap(onesT[:]), data1=_ap(maskT[:]), initial=0.0, op0=ALU.mult, op1=ALU.add),
                             reads=[onesT, maskT], writes=[inclT])
                        S.tt(pmf[:], inclT[:], maskT[:], ALU.mult)
                        S.ts(pmf[:], pmf[:], -1.0, None, op0=ALU.add)
                        S.memset(posmT[:], 0.0)
                        S.cp(posmT[0:NE, :], pmf[:])
                        for c in range(nch):
                            off = (c % 4) * NE
                            S.tr(ppt[:, off:off + NE], pmf[:, c * T:(c + 1) * T], identF[0:NE, 0:NE])
                            if c % 4 == 3 or c == nch - 1:
                                w_ = off + NE
                                c0_ = (c // 4) * 4
                                S.cp(posm_tok[:, c0_:c + 1, :].rr("p c e -> p (c e)"), ppt[:, 0:w_])
                    S.barrier()

                S.phase = f"L{l}s{s}.moeE"
                with contextlib.ExitStack() as st:
                    mcap = max(t_["cap"] for t_ in sets); mnch = max(t_["nch"] for t_ in sets)
                    Sg = ring(st, "Sg", [T, mnch, mcap], BF16, 2)
                    xgT = ring(st, "xgT", [T, KT, mcap], BF16, 2)
                    hT = ring(st, "hT", [T, KT, mcap], BF16, 2)
                    sil = ring(st, "sil", [T, mcap], F32, 2)
                    gsl = ring(st, "gsl", [T, 4], F32, 2)
                    yw = ring(st, "yw", [T, 2, D], BF16, 2)
                    pg = ring(st, "pg", [T, 512], F32, 2, psum=True)
                    pf = ring(st, "pf", [T, 512], F32, 2, psum=True)
                    pd = ring(st, "pd", [T, 512], F32, 2, psum=True)
                    pgs = ps(st, "pgs", [T, 4])
                    for e_ in range(NE):
                        if e_ == 0:
                            wg_, wu_, wd_ = pre_w
                        else:
                            wg_ = wr.next(); S.dma(wg_[:], w_gate[l, e_].rr("(k p) f -> p k f", p=T), eng="pool")
                            wu_ = wr.next(); S.dma(wu_[:], w_up[l, e_].rr("(k p) f -> p k f", p=T), eng="pool")
                            wd_ = wr.next(); S.dma(wd_[:], w_down[l, e_].rr("(k p) f -> p k f", p=T), eng="pool")
                        def eset(ts_, e_, wg_, wu_, wd_):
                            nch = ts_["nch"]; cap = ts_["cap"]; CT = ts_["CT"]; CP = ts_["CP"]
                            xf = ts_["xf"]; affhl = ts_["affhl"]; posm_tok = ts_["posm_tok"]
                            sg_ = Sg.next(); xg = xgT.next(); ht = hT.next(); gs = gsl.next(); yw_ = yw.next()
                            for c in range(nch):
                                S.ts(sg_[:, c, 0:cap], iota_slot[:, 0:cap], posm_tok[:, c, e_:e_ + 1], None, op0=ALU.is_equal)
                            yield
                            for ct in range(CT):
                                for c in range(nch):
                                    S.mm(pgs[0:CP, ct * 2:ct * 2 + 2], sg_[:, c, ct * T:ct * T + CP], affhl[:, c, e_, :],
                                         start=(c == 0), stop=(c == nch - 1))
                            S.red(gs[0:CP, 0:CT], pgs[0:CP, 0:2 * CT].rr("p (a b) -> p a b", b=2), ALU.add)
                            p = None
                            for k in range(KT):
                                p = pg.next() if k % 2 == 0 else p
                                o_ = (k % 2) * 256
                                for c in range(nch):
                                    S.mm(p[:, o_:o_ + cap], xf[:, c, k * T:(k + 1) * T], sg_[:, c, 0:cap],
                                         start=(c == 0), stop=(c == nch - 1))
                                S.cp(xg[:, k, 0:cap], p[:, o_:o_ + cap], "act")
                                if k % 4 == 3:
                                    yield
                            for f in range(KT):
                                p = pf.next(); sl = sil.next()
                                for k in range(KT):
                                    S.mm(p[:, 0:cap], wg_[:, k, f * T:(f + 1) * T], xg[:, k, 0:cap], start=(k == 0), stop=(k == KT - 1))
                                for k in range(KT):
                                    S.mm(p[:, 256:256 + cap], wu_[:, k, f * T:(f + 1) * T], xg[:, k, 0:cap], start=(k == 0), stop=(k == KT - 1))
                                S.act(sl[:, 0:cap], p[:, 0:cap], AF.Silu)
                                S.tt(ht[:, f, 0:cap], sl[:, 0:cap], p[:, 256:256 + cap], ALU.mult)
                                if f % 4 == 3:
                                    yield
                            for ct in range(CT):
                                for dh in range(2):
                                    p = pd.next()
                                    for f in range(KT):
                                        S.mm(p[0:CP, :], ht[:, f, ct * T:ct * T + CP], wd_[:, f, dh * 512:(dh + 1) * 512],
                                             start=(f == 0), stop=(f == KT - 1))
                                    S.act(yw_[0:CP, ct, dh * 512:(dh + 1) * 512], p[0:CP, :], AF.Copy, scale=gs[0:CP, ct:ct + 1])
                            S.dma(ts_["yw"][e_, 0:CT, 0:CP, :].rr("c p d -> p c d"), yw_[0:CP, 0:CT, :])

                        live = [eset(ts_, e_, wg_, wu_, wd_) for ts_ in sets]
                        while live:
                            for g_ in list(live):
                                try:
                                    next(g_)
                                except StopIteration:
                                    live.remove(g_)
                S.barrier()
                wst.close()

                S.phase = f"L{l}s{s}.moeB"
                for ts_ in sets:
                    nch = ts_["nch"]; cap = ts_["cap"]; CT = ts_["CT"]; CP = ts_["CP"]; v = ts_["v"]; tok0 = ts_["tok0"]
                    posmT = ts_["posmT"]; final = ts_["final"]; h_dst_v = ts_["dst"]
                    with contextlib.ExitStack() as st:
                        ywa = sb(st, "ywa", [T, NE, CT, D], BF16)
                        onehotE = sb(st, "onehotE", [T, NE, T], BF16)
                        S.memset(onehotE[:], 0.0, "pool")
                        aff_sel(onehotE[:], [[-1, NE], [0, T]], ALU.not_equal, 1.0, 1)
                        STt = ring(st, "STt", [T, CT, NE * T], BF16, 2)
                        g2b = sb(st, "g2b", [T, D]); fnw = sb(st, "fnw", [T, D])
                        hin = ring(st, "hin4", [T, D], F32, 2); ho = ring(st, "ho4", [T, D], F32, 2)
                        sq = sb(st, "sq4", [T, D]); stat = ring(st, "stat4", [T, 2], F32, 2)
                        ppb = ps(st, "ppb", [T, NE * T]); po = ps(st, "po", [T, D])
                        load_vec_bc(g2b[:], l, v, 5)
                        if final:
                            S.dma(fnw[:], final_norm_w[0:1, :].bc([T, D]))
                        S.dma(ywa[0:CP, :, :, :], ts_["yw"][:, 0:CT, 0:CP, :].rr("e c p d -> p e c d"))

                        def b_s1(c):
                            stt_ = STt.next(); h_ = hin.next()
                            S.dma(h_[:], h_src[tok0 + c * T:tok0 + (c + 1) * T, :])
                            for e_ in range(NE):
                                S.mm(ppb[:, e_ * T:(e_ + 1) * T], onehotE[:, e_, :], posmT[:, c * T:(c + 1) * T])
                            for ct in range(CT):
                                S.ts(stt_[0:CP, ct, :], ppb[0:CP, :], iota_part[0:CP, ct:ct + 1], None, op0=ALU.is_equal)
                            return (stt_, h_)

                        def b_s2(c, cx):
                            stt_, h_ = cx
                            o_ = ho.next(); sa = stat.next()
                            for dh in range(2):
                                i_ = 0
                                for e_ in range(NE):
                                    for ct in range(CT):
                                        S.mm(po[:, dh * 512:(dh + 1) * 512], stt_[0:CP, ct, e_ * T:(e_ + 1) * T],
                                             ywa[0:CP, e_, ct, dh * 512:(dh + 1) * 512],
                                             start=(i_ == 0), stop=(i_ == NE * CT - 1))
                                        i_ += 1
                            S.tt(o_[:], po[:], g2b[:], ALU.mult)
                            S.tt(o_[:], o_[:], h_[:], ALU.add)
                            if final:
                                S.act(sq[:], o_[:], AF.Square, accum=sa[:, 0:1])
                                rms_rstd(None, sa[:, 0:1], D, sa[:, 1:2])
                                S.stt(o_[:], o_[:], sa[:, 1:2], fnw[:], ALU.mult, ALU.mult)
                            S.dma(h_dst_v[c * T:(c + 1) * T, :], o_[:])

                        prev = None
                        for c in range(nch):
                            cx = b_s1(c)
                            if prev is not None:
                                b_s2(*prev)
                            prev = (c, cx)
                        b_s2(*prev)
                    S.barrier()

        for l in range(nlayers):
            last = (l == DEPTH - 1)
            for s in range(nseq):
                if l == 0:
                    src_l, src_c = x_in[s], ctx_in[s]
                else:
                    src_l, src_c = hB[s][NCTX:NTOK, :], hB[s][0:NCTX, :]
                mixer(l, s, src_l, src_c, hA[s], last)
                if stage in ("m1", "m2", "m3", "m4a"):
                    continue
                if stage == "mix":
                    continue
                if not last:
                    sets = [dict(v=2, tok0=0, nch=2, cap=32, dst=hB[s][0:NCTX, :], final=False, yw=ywc[s]),
                            dict(v=s, tok0=NCTX, nch=16, cap=256, dst=hB[s][NCTX:NTOK, :], final=False, yw=ywb[s])]
                else:
                    sets = [dict(v=s, tok0=NCTX, nch=16, cap=256, dst=y_out[s], final=True, yw=ywb[s])]
                moe(l, s, sets, hA[s])

        outs = [y_out]
        if stage != "full":
            with contextlib.ExitStack() as st:
                tmp = ring(st, "dbgt", [T, D], F32, 2)
                for s in range(nseq):
                    srcs = {"m3": ybT[s], "mix": hA[s], "m4a": None, "m1": None, "m2": None, "moe0": hB[s]}.get(stage)
                    if srcs is None:
                        continue
                    for c in range(16):
                        t_ = tmp.next()
                        S.dma(t_[:], srcs[NCTX + c * T:NCTX + (c + 1) * T, :])
                        S.dma(y_out[s, c * T:(c + 1) * T, :], t_[:])
        S.emit(final_waits=[y_out])
    return nc


def _prep_common(inp):
    L = DEPTH
    f = lambda a: np.ascontiguousarray(np.asarray(a, dtype=np.float32))
    com = {
        "ada_w": f(inp["ada_w"]), "ada_b": f(inp["ada_b"]),
        "norm1_w": f(inp["norm1_w"]), "norm2_w": f(inp["norm2_w"]),
        "w_in": f(inp["w_in"]),
        "convw_h": f(np.asarray(inp["conv_w"]).reshape(L, 3, 16, T).transpose(0, 3, 2, 1)),
        "convb_h": f(np.asarray(inp["conv_b"]).reshape(L, 16, T).transpose(0, 2, 1)),
        "dt_bias": f(np.asarray(inp["dt_bias"]).reshape(L, 32)),
        "a_log": f(np.asarray(inp["a_log"]).reshape(L, 32)),
        "d_skip": f(inp["d_skip"]),
        "ssd_norm_w": f(inp["ssd_norm_w"]), "sgu_ln_w": f(inp["sgu_ln_w"]), "sgu_ln_b": f(inp["sgu_ln_b"]),
        "w_s": f(inp["w_s"]), "b_s": f(np.asarray(inp["b_s"]).reshape(L, 8 * T)),
        "w_pa": f(inp["w_pa"]), "w_pb": f(inp["w_pb"]), "w_o": f(inp["w_o"]),
        "router_w": f(inp["router_w"]),
        "w_gate": f(inp["w_gate"]), "w_up": f(inp["w_up"]), "w_down": f(inp["w_down"]),
        "final_norm_w": f(np.asarray(inp["final_norm_w"]).reshape(1, D)),
    }
    return com


def _core_inputs(inp, com, seqs):
    x = np.asarray(inp["x"], dtype=np.float32); ctx = np.asarray(inp["ctx"], dtype=np.float32)
    c = np.asarray(inp["c"], dtype=np.float32); c_ctx = np.asarray(inp["c_ctx"], dtype=np.float32)
    vecs = [c[b] for b in seqs]
    while len(vecs) < 2:
        vecs.append(c[seqs[0]])
    cv = np.stack(vecs + [c_ctx], axis=0)
    cT = np.ascontiguousarray(cv.reshape(3, KT, T).transpose(2, 1, 0))
    m = dict(com)
    m["x"] = np.ascontiguousarray(x[list(seqs)])
    m["ctx"] = np.ascontiguousarray(ctx[list(seqs)])
    m["cT"] = cT
    return m


def kernel(**inputs):
    nseq = 2
    nc = build_nc(nseq=nseq, stage="full")
    com = _prep_common(inputs)
    in_maps = [_core_inputs(inputs, com, (2 * i, 2 * i + 1)) for i in range(NCORES)]
    res = run_bass_kernel_spmd(nc, in_maps, core_ids=list(range(NCORES)))
    out = np.concatenate([np.asarray(r["y"], dtype=np.float32) for r in res.results], axis=0)
    return out
```
